# Optimizing a Trainium2 kernel written in Bass

```python
import math
import jax
import jax.numpy as jnp
from jax import lax
import numpy as np

D_MODEL = 2048
BATCH = 2
SEQ = 16384
DEPTH = 2

HEAD_DIM = 128
A_HEADS = 8
A_KV_HEADS = 2
B_HEADS = 8
B_PATTERNS = ((128, 1), (512, 4), (2048, 16))
C_HEADS = 8
C_VALUE_DIM = 2 * HEAD_DIM
D_FF_DENSE = 5632
N_EXPERTS = 8
TOP_K = 2
D_FF_EXPERT = 7168
MOE_BLOCK = 2048
Q_BLOCK = 128
GRID_W = 64
ROPE_THETA = 10000.0
NORM_EPS = 1e-6
NEG_INF = -1e30
A_Q_DIM = A_HEADS * HEAD_DIM
A_KV_DIM = A_KV_HEADS * HEAD_DIM
B_DIM = B_HEADS * HEAD_DIM
L0_IN_DIM = A_Q_DIM + 2 * A_KV_DIM + 3 * B_DIM
L0_MIX_DIM = A_Q_DIM + B_DIM
C_QK_DIM = C_HEADS * 2 * HEAD_DIM
C_V_DIM = C_HEADS * C_VALUE_DIM
L1_IN_DIM = 2 * C_QK_DIM + C_V_DIM

kernel_name = "hybrid_gqa_dilated_diffattn_moe_encoder"


def rms_norm(x, g):
    xf = x.astype(jnp.float32)
    y = xf * lax.rsqrt(jnp.mean(xf * xf, axis=-1, keepdims=True) + NORM_EPS)
    return (y * g.astype(jnp.float32)).astype(x.dtype)


def alibi_slopes(n_heads):
    return 2.0 ** (-8.0 * jnp.arange(1, n_heads + 1, dtype=jnp.float32) / n_heads)


def axial_rope_angles(S):
    rows = S // GRID_W
    row = jnp.broadcast_to(jnp.arange(rows, dtype=jnp.float32)[:, None], (rows, GRID_W)).reshape(S)
    col = jnp.broadcast_to(jnp.arange(GRID_W, dtype=jnp.float32)[None, :], (rows, GRID_W)).reshape(S)
    n_freq = HEAD_DIM // 4
    inv = ROPE_THETA ** (-jnp.arange(n_freq, dtype=jnp.float32) / n_freq)
    return row[:, None] * inv, col[:, None] * inv


def rope_half(x, ang):
    c = jnp.cos(ang)[:, None, :]
    s = jnp.sin(ang)[:, None, :]
    x1, x2 = jnp.split(x, 2, axis=-1)
    return jnp.concatenate([x1 * c - x2 * s, x2 * c + x1 * s], axis=-1)


def apply_axial_rope(x, ang_row, ang_col):
    xf = x.astype(jnp.float32)
    half = HEAD_DIM // 2
    out = jnp.concatenate([rope_half(xf[..., :half], ang_row), rope_half(xf[..., half:], ang_col)], axis=-1)
    return out.astype(x.dtype)


def gqa_attention(q, k, v):
    B, S, Hq, Dh = q.shape
    Hkv = k.shape[2]
    G = Hq // Hkv
    nb = S // Q_BLOCK
    scale = Dh ** -0.5
    qb = q.reshape(B, nb, Q_BLOCK, Hkv, G, Dh).transpose(1, 0, 2, 3, 4, 5)

    def block(q_blk):
        s = jnp.einsum('bqhgd,bkhd->bhgqk', q_blk, k, preferred_element_type=jnp.float32) * scale
        p = jax.nn.softmax(s, axis=-1)
        return jnp.einsum('bhgqk,bkhd->bqhgd', p.astype(v.dtype), v)

    o = lax.map(block, qb)
    return o.transpose(1, 0, 2, 3, 4, 5).reshape(B, S, Hq, Dh)


def dilated_window_attention(q, k, v, slopes, window, dil):
    B, S, H, Dh = q.shape
    L = S // dil
    R = window // (2 * dil)
    W = R
    nb = -(-L // W)
    Lp = nb * W
    BD = B * dil
    scale = Dh ** -0.5

    def to_sub(t):
        return t.reshape(B, L, dil, H, Dh).transpose(0, 2, 1, 3, 4).reshape(BD, L, H, Dh)

    def key_blocks(t):
        tp = jnp.pad(to_sub(t), ((0, 0), (W, Lp - L + W), (0, 0), (0, 0))).reshape(BD, nb + 2, W, H, Dh)
        return jnp.concatenate([tp[:, :-2], tp[:, 1:-1], tp[:, 2:]], axis=2)

    qs = jnp.pad(to_sub(q), ((0, 0), (0, Lp - L), (0, 0), (0, 0))).reshape(BD, nb, W, H, Dh)
    kb = key_blocks(k)
    vb = key_blocks(v)
    rel = jnp.arange(3 * W)[None, :] - W - jnp.arange(W)[:, None]
    kidx = jnp.arange(nb)[:, None] * W - W + jnp.arange(3 * W)[None, :]
    valid = (jnp.abs(rel) <= R)[None] & ((kidx >= 0) & (kidx < L))[:, None, :]
    bias = -slopes[:, None, None] * (dil * jnp.abs(rel)).astype(jnp.float32)[None]
    s = jnp.einsum('znqhd,znkhd->znhqk', qs, kb, preferred_element_type=jnp.float32) * scale
    s = jnp.where(valid[None, :, None], s + bias[None, None], NEG_INF)
    m = jnp.max(s, axis=-1, keepdims=True)
    p = jnp.exp(s - m)
    l = jnp.sum(p, axis=-1, keepdims=True)
    o = jnp.einsum('znhqk,znkhd->znqhd', (p / l).astype(v.dtype), vb)
    lse = (m + jnp.log(l))[..., 0].transpose(0, 1, 3, 2)

    def from_sub(t):
        t = t[:, :L].reshape((B, dil, L) + t.shape[2:])
        t = jnp.moveaxis(t, 1, 2)
        return t.reshape((B, S) + t.shape[3:])

    return from_sub(o.reshape(BD, Lp, H, Dh)), from_sub(lse.reshape(BD, Lp, H))


def dilated_mixture_attention(q, k, v, slopes):
    outs = []
    lses = []
    for window, dil in B_PATTERNS:
        o_p, lse_p = dilated_window_attention(q, k, v, slopes, window, dil)
        outs.append(o_p)
        lses.append(lse_p)
    wts = jax.nn.softmax(jnp.stack(lses, axis=0), axis=0)
    o = jnp.einsum('pbsh,pbshd->bshd', wts, jnp.stack(outs, axis=0).astype(jnp.float32))
    return o.astype(q.dtype)


def diff_attention(q, k, v, lam, slopes):
    B, S, H, _, Dh = q.shape
    Dv = v.shape[-1]
    nb = S // Q_BLOCK
    scale = Dh ** -0.5
    kpos = jnp.arange(S, dtype=jnp.float32)
    qb = q.reshape(B, nb, Q_BLOCK, H, 2, Dh).transpose(1, 0, 2, 3, 4, 5)

    def block(args):
        q_blk, i = args
        qpos = (i * Q_BLOCK + jnp.arange(Q_BLOCK)).astype(jnp.float32)
        bias = -slopes[:, None, None] * jnp.abs(qpos[:, None] - kpos[None, :])[None]
        s = jnp.einsum('bqhcd,bkhcd->bhcqk', q_blk, k, preferred_element_type=jnp.float32) * scale
        p = jax.nn.softmax(s + bias[None, :, None], axis=-1)
        a = p[:, :, 0] - lam * p[:, :, 1]
        return jnp.einsum('bhqk,bkhd->bqhd', a.astype(v.dtype), v)

    o = lax.map(block, (qb, jnp.arange(nb)))
    return o.transpose(1, 0, 2, 3, 4).reshape(B, S, H, Dv)


def swiglu(h, w_gate, w_up, w_down):
    return (jax.nn.silu(h @ w_gate) * (h @ w_up)) @ w_down


def moe_swiglu(h, w_router, e_gate, e_up, e_down):
    T, D = h.shape
    logits = jnp.einsum('td,de->te', h, w_router, preferred_element_type=jnp.float32)
    top_val, top_idx = lax.top_k(logits, TOP_K)
    gates = jax.nn.softmax(top_val, axis=-1).astype(h.dtype)
    A = T * TOP_K
    a_exp = top_idx.reshape(A).astype(jnp.int32)
    a_tok = jnp.repeat(jnp.arange(T, dtype=jnp.int32), TOP_K)
    a_gate = gates.reshape(A)
    s_exp, s_tok, s_gate = lax.sort((a_exp, a_tok, a_gate), dimension=0, is_stable=True, num_keys=1)
    counts = jnp.bincount(a_exp, length=N_EXPERTS)
    padded = (counts + MOE_BLOCK - 1) // MOE_BLOCK * MOE_BLOCK
    start = jnp.cumsum(counts) - counts
    pend = jnp.cumsum(padded)
    pstart = pend - padded
    dest = pstart[s_exp] + jnp.arange(A, dtype=jnp.int32) - start[s_exp]
    n_blocks = -(-A // MOE_BLOCK) + N_EXPERTS
    P = n_blocks * MOE_BLOCK
    buf_tok = jnp.full((P,), T, jnp.int32).at[dest].set(s_tok)
    buf_gate = jnp.zeros((P,), h.dtype).at[dest].set(s_gate)
    blk_exp = jnp.minimum(jnp.searchsorted(pend, jnp.arange(n_blocks, dtype=jnp.int32) * MOE_BLOCK, side='right'), N_EXPERTS - 1)
    h_pad = jnp.concatenate([h, jnp.zeros((1, D), h.dtype)], axis=0)
    xs = h_pad[buf_tok].reshape(n_blocks, MOE_BLOCK, D)

    def expert_block(args):
        xb, e = args
        return (jax.nn.silu(xb @ e_gate[e]) * (xb @ e_up[e])) @ e_down[e]

    ys = lax.map(expert_block, (xs, blk_exp)).reshape(P, D)
    return jax.ops.segment_sum(ys * buf_gate[:, None], buf_tok, num_segments=T + 1)[:T]


def even_layer(x, norm_mix, w_in, qn_a, kn_a, qn_b, kn_b, w_out, norm_ffn, w_gate, w_up, w_down):
    B, S, _ = x.shape
    h = rms_norm(x, norm_mix)
    proj = jnp.einsum('bsd,de->bse', h, w_in)
    splits = np.cumsum([A_Q_DIM, A_KV_DIM, A_KV_DIM, B_DIM, B_DIM]).tolist()
    q_a, k_a, v_a, q_b, k_b, v_b = jnp.split(proj, splits, axis=-1)
    ang_row, ang_col = axial_rope_angles(S)
    q_a = apply_axial_rope(rms_norm(q_a.reshape(B, S, A_HEADS, HEAD_DIM), qn_a), ang_row, ang_col)
    k_a = apply_axial_rope(rms_norm(k_a.reshape(B, S, A_KV_HEADS, HEAD_DIM), kn_a), ang_row, ang_col)
    o_a = gqa_attention(q_a, k_a, v_a.reshape(B, S, A_KV_HEADS, HEAD_DIM))
    q_b = rms_norm(q_b.reshape(B, S, B_HEADS, HEAD_DIM), qn_b)
    k_b = rms_norm(k_b.reshape(B, S, B_HEADS, HEAD_DIM), kn_b)
    o_b = dilated_mixture_attention(q_b, k_b, v_b.reshape(B, S, B_HEADS, HEAD_DIM), alibi_slopes(B_HEADS))
    mix = jnp.concatenate([o_a.reshape(B, S, A_Q_DIM), o_b.reshape(B, S, B_DIM)], axis=-1)
    x = x + jnp.einsum('bse,ed->bsd', mix, w_out)
    return x + swiglu(rms_norm(x, norm_ffn), w_gate, w_up, w_down)


def odd_layer(x, layer, norm_mix, w_in, qn_c, kn_c, lam_q1, lam_k1, lam_q2, lam_k2, subln, w_out,
              norm_ffn, w_router, e_gate, e_up, e_down):
    B, S, D = x.shape
    h = rms_norm(x, norm_mix)
    proj = jnp.einsum('bsd,de->bse', h, w_in)
    q, k, v = jnp.split(proj, [C_QK_DIM, 2 * C_QK_DIM], axis=-1)
    q = rms_norm(q.reshape(B, S, C_HEADS, 2, HEAD_DIM), qn_c)
    k = rms_norm(k.reshape(B, S, C_HEADS, 2, HEAD_DIM), kn_c)
    v = v.reshape(B, S, C_HEADS, C_VALUE_DIM)
    lambda_init = 0.8 - 0.6 * math.exp(-0.3 * layer)
    f32 = jnp.float32
    lam = (jnp.exp(jnp.sum(lam_q1.astype(f32) * lam_k1.astype(f32)))
           - jnp.exp(jnp.sum(lam_q2.astype(f32) * lam_k2.astype(f32))) + lambda_init)
    o = diff_attention(q, k, v, lam, alibi_slopes(C_HEADS))
    o = rms_norm(o, subln) * (1.0 - lambda_init)
    x = x + jnp.einsum('bse,ed->bsd', o.reshape(B, S, C_V_DIM), w_out)
    hf = rms_norm(x, norm_ffn).reshape(B * S, D)
    return x + moe_swiglu(hf, w_router, e_gate, e_up, e_down).reshape(B, S, D)


def setup_inputs(seed: int = 0) -> dict:
    key = jax.random.key(seed)
    ks = iter(list(jax.random.split(key, 32)))

    def w(shape, fan_in):
        return jax.random.normal(next(ks), shape, jnp.float32) * (fan_in ** -0.5)

    def gain(n):
        return 1.0 + 0.02 * jax.random.normal(next(ks), (n,), jnp.float32)

    def small(shape, s):
        return s * jax.random.normal(next(ks), shape, jnp.float32)

    return {
        'x': jax.random.normal(next(ks), (BATCH, SEQ, D_MODEL), jnp.float32),
        'l0_norm_mix': gain(D_MODEL),
        'l0_w_in': w((D_MODEL, L0_IN_DIM), D_MODEL),
        'l0_qnorm_a': gain(HEAD_DIM),
        'l0_knorm_a': gain(HEAD_DIM),
        'l0_qnorm_b': gain(HEAD_DIM),
        'l0_knorm_b': gain(HEAD_DIM),
        'l0_w_out': w((L0_MIX_DIM, D_MODEL), L0_MIX_DIM),
        'l0_norm_ffn': gain(D_MODEL),
        'l0_w_gate': w((D_MODEL, D_FF_DENSE), D_MODEL),
        'l0_w_up': w((D_MODEL, D_FF_DENSE), D_MODEL),
        'l0_w_down': w((D_FF_DENSE, D_MODEL), D_FF_DENSE),
        'l1_norm_mix': gain(D_MODEL),
        'l1_w_in': w((D_MODEL, L1_IN_DIM), D_MODEL),
        'l1_qnorm_c': gain(HEAD_DIM),
        'l1_knorm_c': gain(HEAD_DIM),
        'l1_lambda_q1': small((HEAD_DIM,), 0.1),
        'l1_lambda_k1': small((HEAD_DIM,), 0.1),
        'l1_lambda_q2': small((HEAD_DIM,), 0.1),
        'l1_lambda_k2': small((HEAD_DIM,), 0.1),
        'l1_subln': gain(C_VALUE_DIM),
        'l1_w_out': w((C_V_DIM, D_MODEL), C_V_DIM),
        'l1_norm_ffn': gain(D_MODEL),
        'l1_w_router': w((D_MODEL, N_EXPERTS), D_MODEL),
        'l1_e_gate': w((N_EXPERTS, D_MODEL, D_FF_EXPERT), D_MODEL),
        'l1_e_up': w((N_EXPERTS, D_MODEL, D_FF_EXPERT), D_MODEL),
        'l1_e_down': w((N_EXPERTS, D_FF_EXPERT, D_MODEL), D_FF_EXPERT),
    }


def reference(x, l0_norm_mix, l0_w_in, l0_qnorm_a, l0_knorm_a, l0_qnorm_b, l0_knorm_b, l0_w_out,
              l0_norm_ffn, l0_w_gate, l0_w_up, l0_w_down,
              l1_norm_mix, l1_w_in, l1_qnorm_c, l1_knorm_c, l1_lambda_q1, l1_lambda_k1,
              l1_lambda_q2, l1_lambda_k2, l1_subln, l1_w_out, l1_norm_ffn, l1_w_router,
              l1_e_gate, l1_e_up, l1_e_down):
    for layer in range(DEPTH):
        if layer % 2 == 0:
            x = even_layer(x, l0_norm_mix, l0_w_in, l0_qnorm_a, l0_knorm_a, l0_qnorm_b, l0_knorm_b,
                           l0_w_out, l0_norm_ffn, l0_w_gate, l0_w_up, l0_w_down)
        else:
            x = odd_layer(x, layer, l1_norm_mix, l1_w_in, l1_qnorm_c, l1_knorm_c, l1_lambda_q1,
                          l1_lambda_k1, l1_lambda_q2, l1_lambda_k2, l1_subln, l1_w_out,
                          l1_norm_ffn, l1_w_router, l1_e_gate, l1_e_up, l1_e_down)
    return x
```

```python
import numpy as np
import concourse.bass as bass
import concourse.mybir as mybir
from concourse.bass_utils import run_bass_kernel_spmd

F32 = mybir.dt.float32
BF16 = mybir.dt.bfloat16
I32 = mybir.dt.int32
ALU = mybir.AluOpType
AF = mybir.ActivationFunctionType
AX = mybir.AxisListType

PE, ACT, DVE, POOL, SP = "pe", "act", "dve", "pool", "sp"
ENGS = (PE, ACT, DVE, POOL, SP)
SAME_ENGINE_SYNC = {PE: False, ACT: True, DVE: True, POOL: True, SP: False}


class Buf:
    def __init__(self, name):
        self.name = name
        self.w = []
        self.r = []
        self.dsem = None
        self.dcnt = 0


class Op:
    __slots__ = ("eng", "fn", "deps", "idx", "signal", "count", "is_dma", "dsem", "dcount", "waits", "inc")

    def __init__(self, eng, fn):
        self.eng = eng
        self.fn = fn
        self.deps = []
        self.idx = -1
        self.signal = False
        self.count = 0
        self.is_dma = False
        self.dsem = None
        self.dcount = 0
        self.waits = []
        self.inc = 16


class Prog:
    def __init__(self, nc):
        self.nc = nc
        self.ops = {e: [] for e in ENGS}
        self.sems = {}
        self.dma_sems = []
        self.old_dma = []
        self.nsem = 0

    def _deps(self, reads, writes, acc=False):
        deps = []
        for b in reads:
            deps.extend(b.w)
        for b in writes:
            deps.extend(b.w)
            deps.extend(b.r)
        return deps

    def _commit(self, op, reads, writes):
        for b in writes:
            b.w = [op]
            b.r = []
        for b in reads:
            if b not in writes:
                key = id(op.dsem) if op.is_dma else op.eng
                b.r = [x for x in b.r if (id(x.dsem) if x.is_dma else x.eng) != key]
                b.r.append(op)

    def op(self, eng, fn, reads=(), writes=()):
        o = Op(eng, fn)
        o.deps = self._deps(reads, writes)
        o.idx = len(self.ops[eng])
        self.ops[eng].append(o)
        self._commit(o, reads, writes)
        return o

    def dma(self, eng, fn, sbuf, reads=(), writes=(), inc=16):
        o = Op(eng, fn)
        o.is_dma = True
        o.deps = self._deps(reads, writes)
        o.idx = len(self.ops[eng])
        o.inc = inc
        if sbuf.dsem is None or sbuf.dcnt + 16 > 30000:
            if sbuf.dsem is not None:
                self.old_dma.append((sbuf.dsem, sbuf.dcnt))
            sbuf.dsem = self.nc.alloc_semaphore(f"d_{sbuf.name}_{self.nsem}")
            self.nsem += 1
            sbuf.dcnt = 0
            if sbuf not in self.dma_sems:
                self.dma_sems.append(sbuf)
        sbuf.dcnt += inc
        o.dsem = sbuf.dsem
        o.dcount = sbuf.dcnt
        self.ops[eng].append(o)
        self._commit(o, reads, writes)
        return o

    def emit(self):
        nc = self.nc
        EPOCH = 30000
        for e in ENGS:
            waited_eng = {f: -1 for f in ENGS}
            waited_dma = {}
            for o in self.ops[e]:
                need_eng = {}
                need_dma = {}
                for d in o.deps:
                    if d.is_dma:
                        k = id(d.dsem)
                        if waited_dma.get(k, 0) >= d.dcount:
                            continue
                        if k not in need_dma or need_dma[k][1] < d.dcount:
                            need_dma[k] = (d.dsem, d.dcount)
                    else:
                        if d.eng == e and not SAME_ENGINE_SYNC[e]:
                            continue
                        if waited_eng[d.eng] >= d.idx:
                            continue
                        if d.eng not in need_eng or need_eng[d.eng].idx < d.idx:
                            need_eng[d.eng] = d
                for f, d in need_eng.items():
                    d.signal = True
                    waited_eng[f] = d.idx
                    o.waits.append(d)
                for k, (s, c) in need_dma.items():
                    waited_dma[k] = c
                    o.waits.append((s, c))
        self._final_waits = []
        for e in ENGS:
            if self.ops[e]:
                last = self.ops[e][-1]
                if not last.is_dma:
                    last.signal = True
                    self._final_waits.append(last)
        for e in ENGS:
            c = 0
            cur = None
            for o in self.ops[e]:
                if o.signal and not o.is_dma:
                    if cur is None or c >= EPOCH:
                        cur = nc.alloc_semaphore(f"eng_{e}_{self.nsem}")
                        self.nsem += 1
                        c = 0
                    c += 1
                    o.count = c
                    o.dsem = cur
        prog = self

        def run(e, eng):
            for o in prog.ops[e]:
                for w in o.waits:
                    if isinstance(w, tuple):
                        eng.wait_ge(w[0], w[1])
                    else:
                        eng.wait_ge(w.dsem, w.count)
                ins = o.fn(eng)
                if o.is_dma:
                    ins.then_inc(o.dsem, o.inc)
                elif o.signal:
                    ins.then_inc(o.dsem, 1)
            if e == SP:
                for last in prog._final_waits:
                    if last.eng != SP:
                        eng.wait_ge(last.dsem, last.count)
                for b in prog.dma_sems:
                    eng.wait_ge(b.dsem, b.dcnt)
                for (sm, ct) in prog.old_dma:
                    eng.wait_ge(sm, ct)

        with nc.Block() as block:
            @block.tensor
            def _(eng):
                run(PE, eng)

            @block.scalar
            def _(eng):
                run(ACT, eng)

            @block.vector
            def _(eng):
                run(DVE, eng)

            @block.gpsimd
            def _(eng):
                run(POOL, eng)

            @block.sync
            def _(eng):
                run(SP, eng)


class Tile:
    def __init__(self, nc, name, shape, dtype, psum=False):
        self.name = name
        if psum:
            self.t = nc.alloc_psum_tensor("ps_" + name, list(shape), dtype)
        else:
            self.t = nc.alloc_sbuf_tensor("sb_" + name, list(shape), dtype)
        self.b = Buf(name)
        self.shape = shape

    def __getitem__(self, idx):
        return self.t[idx]


EPS = 1e-6


class Ring:
    def __init__(self, tiles):
        self.tiles = tiles
        self.i = 0

    def next(self):
        t = self.tiles[self.i % len(self.tiles)]
        self.i += 1
        return t


def mk_ring(nc, name, n, shape, dtype, psum=False):
    return Ring([Tile(nc, f"{name}{i}", shape, dtype, psum=psum) for i in range(n)])


def MM(P, out, lhsT, rhs, start, stop, reads, writes):
    P.op(PE, lambda e: e.matmul(out, lhsT=lhsT, rhs=rhs, start=start, stop=stop), reads, writes)


def build_consts(nc, P):
    identf = Tile(nc, "identf", [128, 128], F32)
    ident = Tile(nc, "ident", [128, 128], BF16)
    ones = Tile(nc, "ones", [128, 128], BF16)
    P.op(POOL, lambda e: e.memset(identf[:], 1.0), writes=[identf.b])
    P.op(POOL, lambda e: e.affine_select(identf[:], identf[:], pattern=[[-1, 128]], compare_op=ALU.is_equal,
                                         fill=0.0, base=0, channel_multiplier=1), reads=[identf.b], writes=[identf.b])
    P.op(DVE, lambda e: e.tensor_copy(ident[:], identf[:]), reads=[identf.b], writes=[ident.b])
    P.op(DVE, lambda e: e.memset(ones[:], 1.0), writes=[ones.b])
    return identf, ident, ones


def norm_transpose(P, nc, C, xt, gB, hb, hT, ptr, nsub):
    junk, ss, rstd, ident = C["junk"], C["ss"], C["rstd"], C["ident"]
    for s in range(nsub):
        P.op(ACT, lambda e, s=s: e.activation(junk[:], xt[:, s, :], AF.Square, accum_out=ss[:, s:s + 1]),
             reads=[xt.b], writes=[junk.b, ss.b])
    P.op(ACT, lambda e: e.activation(rstd[:, 0:nsub], ss[:, 0:nsub], AF.Sqrt, bias=C["epsb"][:, 0:1], scale=1.0 / 2048),
         reads=[ss.b, C["epsb"].b], writes=[rstd.b])
    P.op(DVE, lambda e: e.reciprocal(rstd[:, 0:nsub], rstd[:, 0:nsub]), reads=[rstd.b], writes=[rstd.b])
    for s in range(nsub):
        P.op(DVE, lambda e, s=s: e.scalar_tensor_tensor(out=hb[:, s, :], in0=xt[:, s, :], scalar=rstd[:, s:s + 1],
                                                         in1=gB[:], op0=ALU.mult, op1=ALU.mult),
             reads=[xt.b, rstd.b, gB.b], writes=[hb.b])
    transpose_to_fm(P, C, hb, hT, ptr, nsub)


def transpose_to_fm(P, C, hb, hT, ptr, nsub):
    ident = C["ident"]
    for c in range(16):
        pt = ptr.next()
        for s in range(nsub):
            P.op(PE, lambda e, c=c, s=s, pt=pt: e.transpose(pt[:, s * 128:(s + 1) * 128], hb[:, s, c * 128:(c + 1) * 128], ident[:]),
                 reads=[hb.b, ident.b], writes=[pt.b])
        if c % 2 == 0:
            P.op(ACT, lambda e, c=c, pt=pt: e.copy(hT[:, c, 0:nsub * 128], pt[:, 0:nsub * 128]), reads=[pt.b], writes=[hT.bs[c]])
        else:
            P.op(DVE, lambda e, c=c, pt=pt: e.tensor_copy(hT[:, c, 0:nsub * 128], pt[:, 0:nsub * 128]), reads=[pt.b], writes=[hT.bs[c]])


def k1_program(T, NCOL, chunks, vcols, nq, nk, TT=512):
    nc = bass.Bass("TRN2", target_bir_lowering=False)
    X = nc.dram_tensor("x", [T, 2048], F32, kind="ExternalInput").ap()
    G = nc.dram_tensor("g", [2048], F32, kind="ExternalInput").ap()
    W = nc.dram_tensor("w", [2048, NCOL], F32, kind="ExternalInput").ap()
    GQ = nc.dram_tensor("gq", [128], F32, kind="ExternalInput").ap()
    GK = nc.dram_tensor("gk", [128], F32, kind="ExternalInput").ap()
    has_rope = any(ch[3] for ch in chunks)
    two = any(ch[4] in ("q2", "k2") for ch in chunks)
    if two:
        GQ2 = nc.dram_tensor("gq2", [128], F32, kind="ExternalInput").ap()
        GK2 = nc.dram_tensor("gk2", [128], F32, kind="ExternalInput").ap()
    if has_rope:
        COS = nc.dram_tensor("cosT", [128, T], F32, kind="ExternalInput").ap()
        SIN = nc.dram_tensor("sinT", [128, T], F32, kind="ExternalInput").ap()
        RT = nc.dram_tensor("rt", [128, 128], F32, kind="ExternalInput").ap()
    NV = sum(v[1] for v in vcols)
    QT = nc.dram_tensor("qT", [nq, 128, T], BF16, kind="ExternalOutput").ap()
    KT = nc.dram_tensor("kT", [nk, 128, T], BF16, kind="ExternalOutput").ap()
    V = nc.dram_tensor("v", [T, NV], BF16, kind="ExternalOutput").ap()
    P = Prog(nc)
    identf, ident, ones = build_consts(nc, P)
    C = {"ident": ident, "junk": Tile(nc, "junk", [128, 2048], BF16), "ss": Tile(nc, "ss", [128, 4], F32),
         "rstd": Tile(nc, "rstd", [128, 4], F32), "epsb": Tile(nc, "epsb", [128, 1], F32)}
    P.op(DVE, lambda e: e.memset(C["epsb"][:], EPS), writes=[C["epsb"].b])
    gB = Tile(nc, "gB", [128, 2048], F32)
    P.dma(SP, lambda e: e.dma_start(out=gB[:], in_=G.partition_broadcast(128)), gB.b, writes=[gB.b])
    gains = {}
    glist = [("q", GQ, 128 ** -0.5), ("k", GK, 1.0)]
    if two:
        glist += [("q2", GQ2, 128 ** -0.5), ("k2", GK2, 1.0)]
    for nm, ap, sc in glist:
        t = Tile(nc, "gain_" + nm, [128, 1], F32)
        P.dma(SP, lambda e, t=t, ap=ap: e.dma_start(out=t[:], in_=ap.rearrange("(p o) -> p o", o=1)), t.b, writes=[t.b])
        if sc != 1.0:
            P.op(DVE, lambda e, t=t, sc=sc: e.tensor_scalar(t[:], t[:], sc, None, op0=ALU.mult), reads=[t.b], writes=[t.b])
        gains[nm] = t
    rtf = Tile(nc, "rtf", [128, 128], F32)
    rtb = Tile(nc, "rtb", [128, 128], BF16)
    if has_rope:
        P.dma(SP, lambda e: e.dma_start(out=rtf[:], in_=RT), rtf.b, writes=[rtf.b])
        P.op(DVE, lambda e: e.tensor_copy(rtb[:], rtf[:]), reads=[rtf.b], writes=[rtb.b])

    nsub = TT // 128
    xt = Tile(nc, "xt", [128, nsub, 2048], F32)
    hb = Tile(nc, "hb", [128, nsub, 2048], BF16)
    hT = Tile(nc, "hT", [128, 16, TT], BF16)
    hT.bs = [Buf(f"hT{c}") for c in range(16)]
    cs = mk_ring(nc, "cs", 2, [128, 2, TT], F32)
    wr = mk_ring(nc, "wblk", 3, [128, 16, 512], BF16)
    ptr = mk_ring(nc, "pt", 2, [128, 512], BF16, psum=True)
    psr = mk_ring(nc, "ps", 2, [128, 512], F32, psum=True)
    ssr = mk_ring(nc, "ssb", 2, [128, 512], F32, psum=True)
    rqr = mk_ring(nc, "rq", 2, [128, 512], F32, psum=True)
    sqr = mk_ring(nc, "sq", 2, [128, TT], BF16)
    rsr = mk_ring(nc, "rs", 2, [128, TT], F32)
    qnr = mk_ring(nc, "qn", 2, [128, TT], BF16)
    tar = mk_ring(nc, "ta", 2, [128, TT], F32)
    tbr = mk_ring(nc, "tb", 2, [128, TT], F32)
    outr = mk_ring(nc, "outt", 3, [128, TT], BF16)
    vtr = mk_ring(nc, "vt", 2, [128, nsub, 512], BF16)
    nblk = NCOL // 512
    for tt in range(T // TT):
        t0 = tt * TT
        P.dma(SP, lambda e, t0=t0: e.dma_start(out=xt[:], in_=X[t0:t0 + TT, :].rearrange("(s p) d -> p s d", p=128)),
              xt.b, writes=[xt.b])
        if has_rope:
            cst = cs.next()
            P.dma(SP, lambda e, t0=t0, cst=cst: e.dma_start(out=cst[:, 0, :], in_=COS[:, t0:t0 + TT]), cst.b, writes=[cst.b])
            P.dma(SP, lambda e, t0=t0, cst=cst: e.dma_start(out=cst[:, 1, :], in_=SIN[:, t0:t0 + TT]), cst.b, reads=[cst.b], writes=[cst.b])
        norm_transpose(P, nc, C, xt, gB, hb, hT, ptr, nsub)
        for blk in range(nblk):
            c0 = blk * 512
            wb = wr.next()
            P.dma(POOL, lambda e, wb=wb, c0=c0: e.dma_start(out=wb[:], in_=W[:, c0:c0 + 512].rearrange("(c p) n -> p c n", p=128)),
                  wb.b, writes=[wb.b])
            for (col0, kind, oidx, rope, gname) in [ch for ch in chunks if c0 <= ch[0] < c0 + 512]:
                lc = col0 - c0
                ps = psr.next()
                for c in range(16):
                    MM(P, ps[:, 0:TT], wb[:, c, lc:lc + 128], hT[:, c, :], c == 0, c == 15, [wb.b, hT.bs[c]], [ps.b])
                sq = sqr.next()
                P.op(ACT, lambda e, sq=sq, ps=ps: e.activation(sq[:], ps[:, 0:TT], AF.Square), reads=[ps.b], writes=[sq.b])
                ssb = ssr.next()
                MM(P, ssb[:, 0:TT], ones[:], sq[:], True, True, [ones.b, sq.b], [ssb.b])
                rs = rsr.next()
                P.op(ACT, lambda e, rs=rs, ssb=ssb: e.activation(rs[:], ssb[:, 0:TT], AF.Sqrt, bias=C["epsb"][:, 0:1], scale=1.0 / 128),
                     reads=[ssb.b, C["epsb"].b], writes=[rs.b])
                P.op(DVE, lambda e, rs=rs: e.reciprocal(rs[:], rs[:]), reads=[rs.b], writes=[rs.b])
                gt = gains[gname]
                dst = QT if kind == "q" else KT
                ot = outr.next()
                if not rope:
                    P.op(DVE, lambda e, ot=ot, ps=ps, rs=rs, gt=gt: e.scalar_tensor_tensor(
                        out=ot[:], in0=ps[:, 0:TT], scalar=gt[:, 0:1], in1=rs[:], op0=ALU.mult, op1=ALU.mult),
                        reads=[ps.b, rs.b, gt.b], writes=[ot.b])
                else:
                    qn = qnr.next()
                    P.op(DVE, lambda e, qn=qn, ps=ps, rs=rs, gt=gt: e.scalar_tensor_tensor(
                        out=qn[:], in0=ps[:, 0:TT], scalar=gt[:, 0:1], in1=rs[:], op0=ALU.mult, op1=ALU.mult),
                        reads=[ps.b, rs.b, gt.b], writes=[qn.b])
                    rq = rqr.next()
                    MM(P, rq[:, 0:TT], rtb[:], qn[:], True, True, [rtb.b, qn.b], [rq.b])
                    ta = tar.next()
                    tb = tbr.next()
                    P.op(POOL, lambda e, ta=ta, qn=qn, cst=cst: e.tensor_tensor(ta[:], qn[:], cst[:, 0, :], op=ALU.mult),
                         reads=[qn.b, cst.b], writes=[ta.b])
                    P.op(DVE, lambda e, tb=tb, rq=rq, cst=cst: e.tensor_tensor(tb[:], rq[:, 0:TT], cst[:, 1, :], op=ALU.mult),
                         reads=[rq.b, cst.b], writes=[tb.b])
                    P.op(POOL, lambda e, ot=ot, ta=ta, tb=tb: e.tensor_tensor(ot[:], ta[:], tb[:], op=ALU.add),
                         reads=[ta.b, tb.b], writes=[ot.b])
                P.dma(SP, lambda e, dst=dst, oidx=oidx, t0=t0, ot=ot: e.dma_start(out=dst[oidx, :, t0:t0 + TT], in_=ot[:]),
                      ot.b, reads=[ot.b])
            for (col0, ncols, ocol0) in [v for v in vcols if c0 <= v[0] < c0 + 512]:
                lc = col0 - c0
                vt = vtr.next()
                for s in range(nsub):
                    ps = psr.next()
                    for c in range(16):
                        MM(P, ps[:, 0:ncols], hT[:, c, s * 128:(s + 1) * 128], wb[:, c, lc:lc + ncols], c == 0, c == 15,
                           [wb.b, hT.bs[c]], [ps.b])
                    if s % 2 == 0:
                        P.op(ACT, lambda e, vt=vt, ps=ps, s=s, ncols=ncols: e.copy(vt[:, s, 0:ncols], ps[:, 0:ncols]),
                             reads=[ps.b], writes=[vt.b])
                    else:
                        P.op(DVE, lambda e, vt=vt, ps=ps, s=s, ncols=ncols: e.tensor_copy(vt[:, s, 0:ncols], ps[:, 0:ncols]),
                             reads=[ps.b], writes=[vt.b])
                P.dma(SP, lambda e, vt=vt, t0=t0, ocol0=ocol0, ncols=ncols: e.dma_start(
                    out=V[t0:t0 + TT, ocol0:ocol0 + ncols].rearrange("(s p) n -> p s n", p=128), in_=vt[:, :, 0:ncols]),
                    vt.b, reads=[vt.b])
    P.emit()
    return nc


def l0_spec():
    chunks = []
    for h in range(8):
        chunks.append((h * 128, "q", h, True, "q"))
    for h in range(2):
        chunks.append((1024 + h * 128, "k", h, True, "k"))
    for h in range(8):
        chunks.append((1536 + h * 128, "q", 8 + h, False, "q2"))
    for h in range(8):
        chunks.append((2560 + h * 128, "k", 2 + h, False, "k2"))
    vcols = [(1280, 256, 0), (3584, 512, 256), (4096, 512, 768)]
    return dict(NCOL=4608, chunks=chunks, vcols=vcols, nq=16, nk=10)


def l1_spec():
    chunks = []
    for c in range(16):
        chunks.append((c * 128, "q", c, False, "q"))
    for c in range(16):
        chunks.append((2048 + c * 128, "k", c, False, "k"))
    vcols = [(4096 + i * 512, 512, i * 512) for i in range(4)]
    return dict(NCOL=6144, chunks=chunks, vcols=vcols, nq=16, nk=16)


def rope_tables(pos):
    pos = np.asarray(pos)
    row = (pos // 64).astype(np.float32)
    col = (pos % 64).astype(np.float32)
    nf = 32
    inv = (np.float32(10000.0) ** (-np.arange(nf, dtype=np.float32) / np.float32(nf))).astype(np.float32)
    ar = (row[None, :] * inv[:, None]).astype(np.float32)
    ac = (col[None, :] * inv[:, None]).astype(np.float32)
    ang = np.concatenate([ar, ar, ac, ac], axis=0)
    R = np.zeros((128, 128), np.float32)
    for base in (0, 64):
        for i in range(32):
            R[base + i, base + i + 32] = -1.0
            R[base + 32 + i, base + i] = 1.0
    return np.cos(ang).astype(np.float32), np.sin(ang).astype(np.float32), np.ascontiguousarray(R.T)


LAMBDA_INIT = 0.8 - 0.6 * float(np.exp(-0.3 * 1))


def k2_l0_program(T, S, SL, NJ=None):
    nc = bass.Bass("TRN2", target_bir_lowering=False)
    QT = nc.dram_tensor("qT", [16, 128, T], BF16, kind="ExternalInput").ap()
    KT = nc.dram_tensor("kT", [2, 128, S], BF16, kind="ExternalInput").ap()
    V = nc.dram_tensor("v", [S, 256], BF16, kind="ExternalInput").ap()
    KTL = nc.dram_tensor("kTl", [8, 128, SL], BF16, kind="ExternalInput").ap()
    VL = nc.dram_tensor("vl", [SL, 1024], BF16, kind="ExternalInput").ap()
    KM = nc.dram_tensor("kmask", [128, SL // 128], F32, kind="ExternalInput").ap()
    BB = nc.dram_tensor("biasB", [8, 20, 128, 512], F32, kind="ExternalInput").ap()
    OT = nc.dram_tensor("oT", [16, 128, T], BF16, kind="ExternalOutput").ap()
    P = Prog(nc)
    identf, ident, ones = build_consts(nc, P)
    NKB = S // 128
    NKL = SL // 128
    nj = T // 512 if NJ is None else NJ
    kt = Tile(nc, "kt", [128, max(S, SL)], BF16)
    NKC = 4
    kt.bs = [Buf(f"kt{i}") for i in range(NKC)]
    vt = Tile(nc, "vt", [128, max(NKB, NKL), 128], BF16)
    NVC = 8
    vt.bs = [Buf(f"vt{i}") for i in range(NVC)]
    bias = Tile(nc, "bias", [128, 20, 512], F32)
    bias.bs = [Buf(f"bias{i}") for i in range(4)]
    km = Tile(nc, "km", [128, NKL], F32)
    P.dma(SP, lambda e: e.dma_start(out=km[:], in_=KM), km.b, writes=[km.b])
    qr = mk_ring(nc, "q", 2, [128, 512], BF16)
    tr = mk_ring(nc, "t", 3, [128, 512], F32)
    pr = mk_ring(nc, "p", 4, [128, 512], BF16)
    rlr = mk_ring(nc, "rl", 2, [128, 512], F32)
    outr = mk_ring(nc, "o", 2, [128, 512], BF16)
    st = mk_ring(nc, "st", 2, [128, 512], F32, psum=True)
    otr = mk_ring(nc, "ot", 2, [128, 512], F32, psum=True)
    smr = mk_ring(nc, "sm", 2, [128, 512], F32, psum=True)

    def attend(qchunk, ochunk, kbs, kb_buf, kb_ap, v_buf, v_ap, bias_i=None):
        for j in range(nj):
            t0 = j * 512
            q = qr.next()
            P.dma(SP, lambda e, q=q, t0=t0: e.dma_start(out=q[:], in_=QT[qchunk, :, t0:t0 + 512]), q.b, writes=[q.b])
            ot = otr.next()
            sm = smr.next()
            blocks = kbs(j)
            n = len(blocks)
            pend = None

            def qk(i):
                kb = blocks[i]
                s = st.next()
                MM(P, s[:], kb_ap(kb), q[:], True, True, [kb_buf(kb), q.b], [s.b])
                p = pr.next()
                if bias_i is None:
                    P.op(ACT, lambda e, p=p, s=s: e.activation(p[:], s[:], AF.Exp), reads=[s.b], writes=[p.b])
                else:
                    t = tr.next()
                    bi = bias_i(j, i)
                    P.op(DVE, lambda e, t=t, s=s, bi=bi: e.tensor_tensor(t[:], s[:], bias[:, bi, :], op=ALU.add),
                         reads=[s.b, bias.bs[bi // 5]], writes=[t.b])
                    P.op(ACT, lambda e, p=p, t=t, kb=kb: e.activation(p[:], t[:], AF.Exp, bias=km[:, kb:kb + 1]),
                         reads=[t.b, km.b], writes=[p.b])
                return (i, kb, p)

            pend = qk(0)
            for i in range(n):
                nxt = qk(i + 1) if i + 1 < n else None
                (ii, kb, p) = pend
                MM(P, ot[:], v_ap(kb), p[:], ii == 0, ii == n - 1, [v_buf(kb), p.b], [ot.b])
                MM(P, sm[:], ones[:], p[:], ii == 0, ii == n - 1, [ones.b, p.b], [sm.b])
                pend = nxt
            rl = rlr.next()
            P.op(DVE, lambda e, rl=rl, sm=sm: e.reciprocal(rl[:], sm[:]), reads=[sm.b], writes=[rl.b])
            o = outr.next()
            P.op(DVE, lambda e, o=o, ot=ot, rl=rl: e.tensor_tensor(o[:], ot[:], rl[:], op=ALU.mult), reads=[ot.b, rl.b], writes=[o.b])
            P.dma(SP, lambda e, o=o, t0=t0: e.dma_start(out=OT[ochunk, :, t0:t0 + 512], in_=o[:]), o.b, reads=[o.b])

    for kvh in range(2):
        cw = S // NKC
        for i in range(NKC):
            P.dma(SP, lambda e, i=i, kvh=kvh: e.dma_start(out=kt[:, i * cw:(i + 1) * cw], in_=KT[kvh, :, i * cw:(i + 1) * cw]),
                  kt.bs[i], writes=[kt.bs[i]])
        vb = NKB // NVC
        for i in range(NVC):
            P.dma(SP, lambda e, i=i, kvh=kvh: e.dma_start(
                out=vt[:, i * vb:(i + 1) * vb, :],
                in_=V[i * vb * 128:(i + 1) * vb * 128, kvh * 128:(kvh + 1) * 128].rearrange("(kb p) d -> p kb d", p=128)),
                vt.bs[i], writes=[vt.bs[i]])
        for qh in range(4 * kvh, 4 * kvh + 4):
            attend(qh, qh, lambda j: list(range(NKB)),
                   lambda kb: kt.bs[kb * 128 // cw], lambda kb: kt[:, kb * 128:(kb + 1) * 128],
                   lambda kb: vt.bs[kb // vb], lambda kb: vt[:, kb, :])
    for h in range(8):
        P.dma(SP, lambda e, h=h: e.dma_start(out=kt[:, 0:SL], in_=KTL[h, :, :]), kt.bs[0],
              reads=[b for b in kt.bs], writes=[b for b in kt.bs])
        nvl = 3 if NKL % 3 == 0 else 1
        vlb = NKL // nvl
        for i in range(nvl):
            P.dma(SP, lambda e, h=h, i=i: e.dma_start(out=vt[:, i * vlb:(i + 1) * vlb, :],
                                                 in_=VL[i * vlb * 128:(i + 1) * vlb * 128, h * 128:(h + 1) * 128].rearrange("(kb p) d -> p kb d", p=128)),
                  vt.bs[i], reads=[b for b in vt.bs], writes=[b for b in vt.bs])
        for i in range(4):
            P.dma(SP, lambda e, h=h, i=i: e.dma_start(out=bias[:, i * 5:(i + 1) * 5, :],
                                                      in_=BB[h, i * 5:(i + 1) * 5].rearrange("i p q -> p i q")),
                  bias.bs[i], writes=[bias.bs[i]])
        attend(8 + h, 8 + h, lambda j: [4 * j + i for i in range(20)],
               lambda kb: kt.bs[0], lambda kb: kt[:, kb * 128:(kb + 1) * 128],
               lambda kb: vt.bs[0], lambda kb: vt[:, kb, :], bias_i=lambda j, i: i)
    P.emit()
    return nc


def dil_bias_tables():
    kk = np.arange(128)[:, None]
    qq = np.arange(512)[None, :]
    out = np.zeros((8, 20, 128, 512), np.float32)
    slopes = 2.0 ** (-8.0 * np.arange(1, 9) / 8)
    for i in range(20):
        o = 128 * i - 1024 + kk - qq
        a = np.abs(o)
        c = (a <= 64).astype(np.int32) + ((o % 4 == 0) & (a <= 256)) + ((o % 16 == 0) & (a <= 1024))
        lnc = np.where(c > 0, np.log(np.maximum(c, 1)), -30000.0)
        for h in range(8):
            out[h, i] = (-slopes[h] * a + lnc).astype(np.float32)
    return out


def k2_l1_program(T, S, NJ=None, NH=8):
    nc = bass.Bass("TRN2", target_bir_lowering=False)
    NKB = S // 128
    nj = T // 512 if NJ is None else NJ
    QT = nc.dram_tensor("qT", [16, 128, T], BF16, kind="ExternalInput").ap()
    KT = nc.dram_tensor("kT", [16, 128, S], BF16, kind="ExternalInput").ap()
    V = nc.dram_tensor("v", [S, 2048], BF16, kind="ExternalInput").ap()
    DTd = nc.dram_tensor("DT", [128, (T // 512) * NKB], F32, kind="ExternalInput").ap()
    KQd = nc.dram_tensor("KQ", [128, 512], F32, kind="ExternalInput").ap()
    LAMd = nc.dram_tensor("lamv", [128, 4], F32, kind="ExternalInput").ap()
    SUBd = nc.dram_tensor("subln", [256], F32, kind="ExternalInput").ap()
    OT = nc.dram_tensor("oT", [16, 128, T], BF16, kind="ExternalOutput").ap()
    P = Prog(nc)
    identf, ident, ones = build_consts(nc, P)
    onesf = Tile(nc, "onesf", [128, 128], F32)
    P.op(POOL, lambda e: e.memset(onesf[:], 1.0), writes=[onesf.b])
    epsb = Tile(nc, "epsb", [128, 1], F32)
    P.op(DVE, lambda e: e.memset(epsb[:], EPS), writes=[epsb.b])
    dt = Tile(nc, "dt", [128, (T // 512) * NKB], F32)
    kq = Tile(nc, "kq", [128, 512], F32)
    lamv = Tile(nc, "lamv", [128, 4], F32)
    sub = Tile(nc, "sub", [128, 2], F32)
    P.dma(SP, lambda e: e.dma_start(out=dt[:], in_=DTd), dt.b, writes=[dt.b])
    P.dma(SP, lambda e: e.dma_start(out=kq[:], in_=KQd), kq.b, writes=[kq.b])
    P.dma(SP, lambda e: e.dma_start(out=lamv[:], in_=LAMd), lamv.b, writes=[lamv.b])
    P.dma(SP, lambda e: e.dma_start(out=sub[:, 0:1], in_=SUBd[0:128].rearrange("(p o) -> p o", o=1)), sub.b, writes=[sub.b])
    P.dma(SP, lambda e: e.dma_start(out=sub[:, 1:2], in_=SUBd[128:256].rearrange("(p o) -> p o", o=1)), sub.b, writes=[sub.b])
    P.op(DVE, lambda e: e.tensor_scalar(sub[:], sub[:], 1.0 - LAMBDA_INIT, None, op0=ALU.mult), reads=[sub.b], writes=[sub.b])
    pr2 = Tile(nc, "pr2", [128, 2], F32)
    neglam = Tile(nc, "neglam", [128, 1], F32)
    st = mk_ring(nc, "st", 2, [128, 512], F32, psum=True)
    P.op(DVE, lambda e: e.tensor_tensor(pr2[:, 0:1], lamv[:, 0:1], lamv[:, 1:2], op=ALU.mult), reads=[lamv.b], writes=[pr2.b])
    P.op(DVE, lambda e: e.tensor_tensor(pr2[:, 1:2], lamv[:, 2:3], lamv[:, 3:4], op=ALU.mult), reads=[lamv.b, pr2.b], writes=[pr2.b])
    s0 = st.next()
    MM(P, s0[:, 0:2], onesf[:], pr2[:], True, True, [onesf.b, pr2.b], [s0.b])
    P.op(ACT, lambda e: e.activation(pr2[:], s0[:, 0:2], AF.Exp), reads=[s0.b], writes=[pr2.b])
    P.op(DVE, lambda e: e.tensor_tensor(neglam[:], pr2[:, 1:2], pr2[:, 0:1], op=ALU.subtract), reads=[pr2.b], writes=[neglam.b])
    P.op(DVE, lambda e: e.tensor_scalar(neglam[:], neglam[:], -LAMBDA_INIT, None, op0=ALU.add), reads=[neglam.b], writes=[neglam.b])

    kt = Tile(nc, "kt", [128, 2, S], BF16)
    NKC = 4
    kt.bs = [[Buf(f"kt{c}_{i}") for i in range(NKC)] for c in range(2)]
    vt = Tile(nc, "vt", [128, NKB, 256], BF16)
    NVC = 8
    vt.bs = [Buf(f"vt{i}") for i in range(NVC)]
    qr = mk_ring(nc, "q", 2, [128, 2, 512], BF16)
    abr = mk_ring(nc, "ab", 3, [128, 512], F32)
    tr = mk_ring(nc, "t", 3, [128, 512], F32)
    pr = mk_ring(nc, "p", 4, [128, 512], BF16)
    ot = [Tile(nc, f"ot{i}", [128, 512], F32, psum=True) for i in range(4)]
    sm = [Tile(nc, f"sm{i}", [128, 512], F32, psum=True) for i in range(2)]
    rl = [Tile(nc, f"rl{i}", [128, 512], F32) for i in range(2)]
    oc = [Tile(nc, f"oc{i}", [128, 512], F32) for i in range(4)]
    dd = [Tile(nc, f"dd{i}", [128, 512], F32) for i in range(2)]
    sq = [Tile(nc, f"sq{i}", [128, 512], BF16) for i in range(2)]
    rs = Tile(nc, "rs", [128, 512], F32)
    outr = mk_ring(nc, "o", 4, [128, 512], BF16)
    slopes = [2.0 ** (-(h + 1)) for h in range(8)]
    cw = S // NKC
    vb = NKB // NVC
    for h in range(NH):
        for c in range(2):
            for i in range(NKC):
                P.dma(SP, lambda e, i=i, c=c, h=h: e.dma_start(out=kt[:, c, i * cw:(i + 1) * cw], in_=KT[2 * h + c, :, i * cw:(i + 1) * cw]),
                      kt.bs[c][i], writes=[kt.bs[c][i]])
        for i in range(NVC):
            P.dma(SP, lambda e, i=i, h=h: e.dma_start(
                out=vt[:, i * vb:(i + 1) * vb, :],
                in_=V[i * vb * 128:(i + 1) * vb * 128, h * 256:(h + 1) * 256].rearrange("(kb p) d -> p kb d", p=128)),
                vt.bs[i], writes=[vt.bs[i]])
        for j in range(nj):
            t0 = j * 512
            q = qr.next()
            P.dma(SP, lambda e, q=q, t0=t0, h=h: e.dma_start(out=q[:], in_=QT[2 * h:2 * h + 2, :, t0:t0 + 512].rearrange("c p t -> p c t")),
                  q.b, writes=[q.b])

            def qk(kb, c, ab):
                s = st.next()
                MM(P, s[:], kt[:, c, kb * 128:(kb + 1) * 128], q[:, c, :], True, True, [kt.bs[c][kb * 128 // cw], q.b], [s.b])
                t = tr.next()
                P.op(DVE, lambda e, t=t, s=s, ab=ab, h=h: e.scalar_tensor_tensor(out=t[:], in0=ab[:], scalar=-slopes[h], in1=s[:],
                                                                              op0=ALU.mult, op1=ALU.add),
                     reads=[ab.b, s.b], writes=[t.b])
                p = pr.next()
                P.op(ACT, lambda e, p=p, t=t: e.activation(p[:], t[:], AF.Exp), reads=[t.b], writes=[p.b])
                return p

            def mkab(kb):
                ab = abr.next()
                idx = j * NKB + kb
                P.op(ACT, lambda e, ab=ab, idx=idx: e.activation(ab[:], kq[:], AF.Abs, bias=dt[:, idx:idx + 1], scale=1.0),
                     reads=[kq.b, dt.b], writes=[ab.b])
                return ab

            def pv(kb, c, p):
                first, last = kb == 0, kb == NKB - 1
                MM(P, ot[2 * c][:], vt[:, kb, 0:128], p[:], first, last, [vt.bs[kb // vb], p.b], [ot[2 * c].b])
                MM(P, ot[2 * c + 1][:], vt[:, kb, 128:256], p[:], first, last, [vt.bs[kb // vb], p.b], [ot[2 * c + 1].b])
                MM(P, sm[c][:], ones[:], p[:], first, last, [ones.b, p.b], [sm[c].b])

            ab = mkab(0)
            pend = [qk(0, 0, ab), qk(0, 1, ab)]
            for kb in range(NKB):
                nxt = [None, None]
                abn = mkab(kb + 1) if kb + 1 < NKB else None
                for c in range(2):
                    if abn is not None:
                        nxt[c] = qk(kb + 1, c, abn)
                    pv(kb, c, pend[c])
                pend = nxt
            for c in range(2):
                P.op(DVE, lambda e, c=c: e.reciprocal(rl[c][:], sm[c][:]), reads=[sm[c].b], writes=[rl[c].b])
                for hf in range(2):
                    i = 2 * c + hf
                    P.op(DVE, lambda e, i=i, c=c: e.tensor_tensor(oc[i][:], ot[i][:], rl[c][:], op=ALU.mult),
                         reads=[ot[i].b, rl[c].b], writes=[oc[i].b])
            ssb = st.next()
            for hf in range(2):
                P.op(DVE, lambda e, hf=hf: e.scalar_tensor_tensor(out=dd[hf][:], in0=oc[2 + hf][:], scalar=neglam[:, 0:1], in1=oc[hf][:],
                                                                 op0=ALU.mult, op1=ALU.add),
                     reads=[oc[2 + hf].b, oc[hf].b, neglam.b], writes=[dd[hf].b])
                P.op(ACT, lambda e, hf=hf: e.activation(sq[hf][:], dd[hf][:], AF.Square), reads=[dd[hf].b], writes=[sq[hf].b])
                MM(P, ssb[:], ones[:], sq[hf][:], hf == 0, hf == 1, [ones.b, sq[hf].b], [ssb.b])
            P.op(ACT, lambda e, ssb=ssb: e.activation(rs[:], ssb[:], AF.Sqrt, bias=epsb[:, 0:1], scale=1.0 / 256),
                 reads=[ssb.b, epsb.b], writes=[rs.b])
            P.op(DVE, lambda e: e.reciprocal(rs[:], rs[:]), reads=[rs.b], writes=[rs.b])
            for hf in range(2):
                o = outr.next()
                P.op(DVE, lambda e, o=o, hf=hf: e.scalar_tensor_tensor(out=o[:], in0=dd[hf][:], scalar=sub[:, hf:hf + 1], in1=rs[:],
                                                                      op0=ALU.mult, op1=ALU.mult),
                     reads=[dd[hf].b, sub.b, rs.b], writes=[o.b])
                P.dma(SP, lambda e, o=o, t0=t0, hf=hf, h=h: e.dma_start(out=OT[2 * h + hf, :, t0:t0 + 512], in_=o[:]), o.b, reads=[o.b])
    P.emit()
    return nc


def k3_program(T, NF, mode, TT=512):
    nc = bass.Bass("TRN2", target_bir_lowering=False)
    if mode != "ffn":
        X = nc.dram_tensor("x", [T, 2048], F32, kind="ExternalInput").ap()
        OTd = nc.dram_tensor("oT", [16, 128, T], BF16, kind="ExternalInput").ap()
        WO = nc.dram_tensor("wo", [2048, 2048], F32, kind="ExternalInput").ap()
        G = nc.dram_tensor("g", [2048], F32, kind="ExternalInput").ap()
    if mode != "router":
        WG = nc.dram_tensor("wg", [1, 2048, NF], F32, kind="ExternalInput").ap()
        WU = nc.dram_tensor("wu", [1, 2048, NF], F32, kind="ExternalInput").ap()
        WD = nc.dram_tensor("wd", [1, NF, 2048], F32, kind="ExternalInput").ap()
    if mode == "router":
        WR = nc.dram_tensor("wr", [2048, 8], F32, kind="ExternalInput").ap()
        HBo = nc.dram_tensor("hb", [T, 2048], BF16, kind="ExternalOutput").ap()
        GTo = nc.dram_tensor("gate", [T, 8], F32, kind="ExternalOutput").ap()
    if mode == "ffn":
        HBi = nc.dram_tensor("hb", [T, 2048], BF16, kind="ExternalInput").ap()
    Y = nc.dram_tensor("y", [T, 2048], F32, kind="ExternalOutput").ap()
    P = Prog(nc)
    identf, ident, ones = build_consts(nc, P)
    C = {"ident": ident, "junk": Tile(nc, "junk", [128, 2048], BF16), "ss": Tile(nc, "ss", [128, 4], F32),
         "rstd": Tile(nc, "rstd", [128, 4], F32), "epsb": Tile(nc, "epsb", [128, 1], F32)}
    P.op(DVE, lambda e: e.memset(C["epsb"][:], EPS), writes=[C["epsb"].b])
    nsub = TT // 128
    NFC = NF // 128
    nb = NFC // 4
    hb = Tile(nc, "hb", [128, nsub, 2048], BF16)
    hT = Tile(nc, "hT", [128, 16, TT], BF16)
    hT.bs = [Buf(f"hT{c}") for c in range(16)]
    wr_ = mk_ring(nc, "wblk", 3, [128, 16, 512], BF16)
    ptr = mk_ring(nc, "pt", 2, [128, 512], BF16, psum=True)
    psr = mk_ring(nc, "ps", 4, [128, 512], F32, psum=True)
    xt = Tile(nc, "xt", [128, nsub, 2048], F32)
    if mode != "ffn":
        gB = Tile(nc, "gB", [128, 2048], F32)
        P.dma(SP, lambda e: e.dma_start(out=gB[:], in_=G.partition_broadcast(128)), gB.b, writes=[gB.b])
    if mode != "router":
        A = Tile(nc, "A", [128, NFC, TT], BF16)
        A.bs = [Buf(f"A{c}") for c in range(NFC)]
        sgr = mk_ring(nc, "sg", 3, [128, TT], F32)
    if mode == "router":
        wrt = Tile(nc, "wrt", [128, 16, 8], F32)
        P.dma(SP, lambda e: e.dma_start(out=wrt[:], in_=WR.rearrange("(c p) n -> p c n", p=128)), wrt.b, writes=[wrt.b])
        hf = Tile(nc, "hf", [128, 2048], F32)
        hTf = Tile(nc, "hTf", [128, 16, 128], F32)
        pf = Tile(nc, "pf", [128, 512], F32, psum=True)
        pl = Tile(nc, "pl", [128, 512], F32, psum=True)
        lg = Tile(nc, "lg", [128, 8], F32)
        m8 = Tile(nc, "m8", [128, 8], F32)
        gs = Tile(nc, "gs", [128, 4], F32)
        eq = Tile(nc, "eq", [128, 16], F32)
        gate = Tile(nc, "gate", [128, nsub, 8], F32)

    def wload(src_ap, nchunk):
        wb = wr_.next()
        P.dma(POOL, lambda e, wb=wb: e.dma_start(out=wb[:, 0:nchunk, :], in_=src_ap), wb.b, writes=[wb.b])
        return wb

    for tt in range(T // TT):
        t0 = tt * TT
        if mode == "ffn":
            P.dma(SP, lambda e, t0=t0: e.dma_start(out=hb[:], in_=HBi[t0:t0 + TT, :].rearrange("(s p) d -> p s d", p=128)),
                  hb.b, writes=[hb.b])
            transpose_to_fm(P, C, hb, hT, ptr, nsub)
        else:
            P.dma(SP, lambda e, t0=t0: e.dma_start(out=xt[:], in_=X[t0:t0 + TT, :].rearrange("(s p) d -> p s d", p=128)),
                  xt.b, writes=[xt.b])
            P.dma(SP, lambda e, t0=t0: e.dma_start(out=hT[:], in_=OTd[:, :, t0:t0 + TT].rearrange("c p t -> p c t")),
                  hT.bs[0], writes=list(hT.bs))
            for cg in range(4):
                wb = wload(WO[:, cg * 512:(cg + 1) * 512].rearrange("(c p) n -> p c n", p=128), 16)
                for s in range(nsub):
                    ps = psr.next()
                    for c in range(16):
                        MM(P, ps[:], hT[:, c, s * 128:(s + 1) * 128], wb[:, c, :], c == 0, c == 15, [wb.b, hT.bs[c]], [ps.b])
                    P.op(DVE, lambda e, ps=ps, s=s, cg=cg: e.tensor_tensor(xt[:, s, cg * 512:(cg + 1) * 512], xt[:, s, cg * 512:(cg + 1) * 512],
                                                                         ps[:], op=ALU.add), reads=[ps.b, xt.b], writes=[xt.b])
            norm_transpose(P, nc, C, xt, gB, hb, hT, ptr, nsub)
        if mode == "router":
            for s in range(nsub):
                P.op(DVE, lambda e, s=s: e.scalar_tensor_tensor(out=hf[:], in0=xt[:, s, :], scalar=C["rstd"][:, s:s + 1], in1=gB[:],
                                                             op0=ALU.mult, op1=ALU.mult), reads=[xt.b, C["rstd"].b, gB.b], writes=[hf.b])
                for g4 in range(4):
                    for i in range(4):
                        c = g4 * 4 + i
                        P.op(PE, lambda e, c=c, i=i: e.transpose(pf[:, i * 128:(i + 1) * 128], hf[:, c * 128:(c + 1) * 128], identf[:]),
                             reads=[hf.b, identf.b], writes=[pf.b])
                    P.op(ACT, lambda e, g4=g4: e.copy(hTf[:, g4 * 4:(g4 + 1) * 4, :], pf[:].rearrange("p (a b) -> p a b", a=4)),
                         reads=[pf.b], writes=[hTf.b])
                for c in range(16):
                    MM(P, pl[:, 0:8], hTf[:, c, :], wrt[:, c, :], c == 0, c == 15, [hTf.b, wrt.b], [pl.b])
                P.op(DVE, lambda e: e.tensor_copy(lg[:], pl[:, 0:8]), reads=[pl.b], writes=[lg.b])
                P.op(DVE, lambda e: e.max(m8[:], lg[:]), reads=[lg.b], writes=[m8.b])
                P.op(DVE, lambda e: e.tensor_tensor(gs[:, 0:1], m8[:, 1:2], m8[:, 0:1], op=ALU.subtract), reads=[m8.b], writes=[gs.b])
                P.op(ACT, lambda e: e.activation(gs[:, 0:1], gs[:, 0:1], AF.Exp), reads=[gs.b], writes=[gs.b])
                P.op(DVE, lambda e: e.tensor_scalar(gs[:, 1:2], gs[:, 0:1], 1.0, None, op0=ALU.add), reads=[gs.b], writes=[gs.b])
                P.op(DVE, lambda e: e.reciprocal(gs[:, 1:2], gs[:, 1:2]), reads=[gs.b], writes=[gs.b])
                P.op(DVE, lambda e: e.tensor_tensor(gs[:, 2:3], gs[:, 0:1], gs[:, 1:2], op=ALU.mult), reads=[gs.b], writes=[gs.b])
                P.op(DVE, lambda e: e.tensor_scalar(eq[:, 0:8], lg[:], m8[:, 0:1], gs[:, 1:2], op0=ALU.is_equal, op1=ALU.mult),
                     reads=[lg.b, m8.b, gs.b], writes=[eq.b])
                P.op(DVE, lambda e: e.tensor_scalar(eq[:, 8:16], lg[:], m8[:, 1:2], gs[:, 2:3], op0=ALU.is_equal, op1=ALU.mult),
                     reads=[lg.b, m8.b, gs.b, eq.b], writes=[eq.b])
                P.op(DVE, lambda e, s=s: e.tensor_tensor(gate[:, s, :], eq[:, 0:8], eq[:, 8:16], op=ALU.add), reads=[eq.b], writes=[gate.b])
            P.dma(SP, lambda e, t0=t0: e.dma_start(out=HBo[t0:t0 + TT, :].rearrange("(s p) d -> p s d", p=128), in_=hb[:]),
                  hb.b, reads=[hb.b])
            P.dma(SP, lambda e, t0=t0: e.dma_start(out=GTo[t0:t0 + TT, :].rearrange("(s p) d -> p s d", p=128), in_=gate[:]),
                  gate.b, reads=[gate.b])
        else:
            for fb in range(NF // 512):
                wgb = wload(WG[0, :, fb * 512:(fb + 1) * 512].rearrange("(c p) n -> p c n", p=128), 16)
                wub = wload(WU[0, :, fb * 512:(fb + 1) * 512].rearrange("(c p) n -> p c n", p=128), 16)
                for fc in range(4):
                    pg = psr.next()
                    for c in range(16):
                        MM(P, pg[:, 0:TT], wgb[:, c, fc * 128:(fc + 1) * 128], hT[:, c, :], c == 0, c == 15, [wgb.b, hT.bs[c]], [pg.b])
                    pu = psr.next()
                    for c in range(16):
                        MM(P, pu[:, 0:TT], wub[:, c, fc * 128:(fc + 1) * 128], hT[:, c, :], c == 0, c == 15, [wub.b, hT.bs[c]], [pu.b])
                    sg = sgr.next()
                    P.op(ACT, lambda e, sg=sg, pg=pg: e.activation(sg[:], pg[:, 0:TT], AF.Silu), reads=[pg.b], writes=[sg.b])
                    ac = fb * 4 + fc
                    P.op(DVE, lambda e, sg=sg, pu=pu, ac=ac: e.tensor_tensor(A[:, ac, :], sg[:], pu[:, 0:TT], op=ALU.mult),
                         reads=[sg.b, pu.b], writes=[A.bs[ac]])
            for cg in range(4):
                accs = [psr.next() for _ in range(nsub)]
                for rb in range(4):
                    wdb = wload(WD[0, rb * nb * 128:(rb + 1) * nb * 128, cg * 512:(cg + 1) * 512].rearrange("(c p) n -> p c n", p=128), nb)
                    for s in range(nsub):
                        for c in range(nb):
                            ac = rb * nb + c
                            MM(P, accs[s][:], A[:, ac, s * 128:(s + 1) * 128], wdb[:, c, :], rb == 0 and c == 0, rb == 3 and c == nb - 1,
                               [wdb.b, A.bs[ac]], [accs[s].b])
                for s in range(nsub):
                    acc = accs[s]
                    if mode == "ffn":
                        if s % 2 == 0:
                            P.op(ACT, lambda e, acc=acc, s=s, cg=cg: e.copy(xt[:, s, cg * 512:(cg + 1) * 512], acc[:]),
                                 reads=[acc.b, xt.b], writes=[xt.b])
                        else:
                            P.op(DVE, lambda e, acc=acc, s=s, cg=cg: e.tensor_copy(xt[:, s, cg * 512:(cg + 1) * 512], acc[:]),
                                 reads=[acc.b, xt.b], writes=[xt.b])
                    else:
                        P.op(DVE, lambda e, acc=acc, s=s, cg=cg: e.tensor_tensor(
                            xt[:, s, cg * 512:(cg + 1) * 512], xt[:, s, cg * 512:(cg + 1) * 512], acc[:], op=ALU.add),
                            reads=[acc.b, xt.b], writes=[xt.b])
        P.dma(SP, lambda e, t0=t0: e.dma_start(out=Y[t0:t0 + TT, :].rearrange("(s p) d -> p s d", p=128), in_=xt[:]),
              xt.b, reads=[xt.b])
    P.emit()
    return nc


def combine_program(T, TT=512):
    nc = bass.Bass("TRN2", target_bir_lowering=False)
    X = nc.dram_tensor("x", [T, 2048], F32, kind="ExternalInput").ap()
    Y1 = nc.dram_tensor("y1", [T, 2048], F32, kind="ExternalInput").ap()
    Y2 = nc.dram_tensor("y2", [T, 2048], F32, kind="ExternalInput").ap()
    GG = nc.dram_tensor("g2", [T, 2], F32, kind="ExternalInput").ap()
    Y = nc.dram_tensor("y", [T, 2048], F32, kind="ExternalOutput").ap()
    P = Prog(nc)
    nsub = TT // 128
    xr = mk_ring(nc, "x", 2, [128, nsub, 2048], F32)
    ar = mk_ring(nc, "a", 2, [128, nsub, 2048], F32)
    gr = mk_ring(nc, "g", 2, [128, nsub, 2], F32)
    for tt in range(T // TT):
        t0 = tt * TT
        xt, gt = xr.next(), gr.next()
        P.dma(SP, lambda e, t0=t0, xt=xt: e.dma_start(out=xt[:], in_=X[t0:t0 + TT, :].rearrange("(s p) d -> p s d", p=128)), xt.b, writes=[xt.b])
        P.dma(SP, lambda e, t0=t0, gt=gt: e.dma_start(out=gt[:], in_=GG[t0:t0 + TT, :].rearrange("(s p) d -> p s d", p=128)), gt.b, writes=[gt.b])
        for k, src in enumerate((Y1, Y2)):
            at = ar.next()
            P.dma(SP, lambda e, t0=t0, at=at, src=src: e.dma_start(out=at[:], in_=src[t0:t0 + TT, :].rearrange("(s p) d -> p s d", p=128)),
                  at.b, writes=[at.b])
            for s in range(nsub):
                P.op(DVE, lambda e, at=at, xt=xt, gt=gt, s=s, k=k: e.scalar_tensor_tensor(
                    out=xt[:, s, :], in0=at[:, s, :], scalar=gt[:, s, k:k + 1], in1=xt[:, s, :], op0=ALU.mult, op1=ALU.add),
                    reads=[at.b, gt.b, xt.b], writes=[xt.b])
        P.dma(SP, lambda e, t0=t0, xt=xt: e.dma_start(out=Y[t0:t0 + TT, :].rearrange("(s p) d -> p s d", p=128), in_=xt[:]), xt.b, reads=[xt.b])
    P.emit()
    return nc


import ml_dtypes

NCORES = 8
TPC = 4096
SEQ = 16384


def _run(nc, in_maps):
    res = run_bass_kernel_spmd(nc, in_maps, core_ids=list(range(NCORES)))
    return res.results


def _gather_batch(rs, key, axis):
    out = []
    for b in range(2):
        out.append(np.concatenate([rs[4 * b + q][key] for q in range(4)], axis=axis))
    return out


def kernel(**inp):
    f32 = np.float32
    x = np.ascontiguousarray(inp["x"], dtype=f32).reshape(NCORES * TPC, 2048)
    xs = [x[c * TPC:(c + 1) * TPC] for c in range(NCORES)]
    tabs = [rope_tables((c % 4) * TPC + np.arange(TPC)) for c in range(4)]
    nc = k1_program(TPC, **l0_spec())
    r1 = _run(nc, [dict(x=xs[c], g=inp["l0_norm_mix"], w=inp["l0_w_in"], gq=inp["l0_qnorm_a"], gk=inp["l0_knorm_a"],
                        gq2=inp["l0_qnorm_b"], gk2=inp["l0_knorm_b"], cosT=tabs[c % 4][0], sinT=tabs[c % 4][1], rt=tabs[c % 4][2])
                   for c in range(NCORES)])
    kT_b = _gather_batch(r1, "kT", 2)
    v_b = _gather_batch(r1, "v", 0)
    biasB = dil_bias_tables()
    SL = TPC + 2048
    maps = []
    for c in range(NCORES):
        b, q = c // 4, c % 4
        lo = q * TPC - 1024
        kTl = np.zeros((8, 128, SL), ml_dtypes.bfloat16)
        vl = np.zeros((SL, 1024), ml_dtypes.bfloat16)
        kmask = np.full((128, SL // 128), -30000.0, f32)
        a0, a1 = max(lo, 0), min(lo + SL, SEQ)
        kTl[:, :, a0 - lo:a1 - lo] = kT_b[b][2:10, :, a0:a1]
        vl[a0 - lo:a1 - lo] = v_b[b][a0:a1, 256:1280]
        kmask[:, (a0 - lo) // 128:(a1 - lo) // 128] = 0.0
        maps.append(dict(qT=r1[c]["qT"], kT=np.ascontiguousarray(kT_b[b][0:2]), v=np.ascontiguousarray(v_b[b][:, 0:256]),
                         kTl=kTl, vl=vl, kmask=kmask, biasB=biasB))
    nc = k2_l0_program(TPC, SEQ, SL)
    r2 = _run(nc, maps)
    nc = k3_program(TPC, 5632, "dense")
    r3 = _run(nc, [dict(x=xs[c], oT=r2[c]["oT"], wo=inp["l0_w_out"], g=inp["l0_norm_ffn"], wg=inp["l0_w_gate"][None],
                        wu=inp["l0_w_up"][None], wd=inp["l0_w_down"][None]) for c in range(NCORES)])
    x1 = [r3[c]["y"] for c in range(NCORES)]
    nc = k1_program(TPC, **l1_spec())
    r4 = _run(nc, [dict(x=x1[c], g=inp["l1_norm_mix"], w=inp["l1_w_in"], gq=inp["l1_qnorm_c"], gk=inp["l1_knorm_c"])
                   for c in range(NCORES)])
    kT_b = _gather_batch(r4, "kT", 2)
    v_b = _gather_batch(r4, "v", 0)
    KQ = (np.arange(128)[:, None] - np.arange(512)[None, :]).astype(f32)
    lamv = np.stack([inp["l1_lambda_q1"], inp["l1_lambda_k1"], inp["l1_lambda_q2"], inp["l1_lambda_k2"]], axis=1).astype(f32)
    maps = []
    for c in range(NCORES):
        b, q = c // 4, c % 4
        DT = np.zeros((128, 8 * 128), f32)
        for j in range(8):
            DT[:, j * 128:(j + 1) * 128] = (np.arange(128) * 128 - (q * TPC + j * 512))[None, :]
        maps.append(dict(qT=r4[c]["qT"], kT=kT_b[b], v=v_b[b], DT=DT, KQ=KQ, lamv=np.ascontiguousarray(lamv), subln=inp["l1_subln"]))
    nc = k2_l1_program(TPC, SEQ)
    r5 = _run(nc, maps)
    nc = k3_program(TPC, 7168, "router")
    r6 = _run(nc, [dict(x=x1[c], oT=r5[c]["oT"], wo=inp["l1_w_out"], g=inp["l1_norm_ffn"], wr=inp["l1_w_router"])
                   for c in range(NCORES)])
    H = np.concatenate([r6[c]["hb"] for c in range(NCORES)], axis=0)
    GT = np.concatenate([r6[c]["gate"] for c in range(NCORES)], axis=0)
    sel = GT != 0
    toks = [np.nonzero(sel[:, e])[0] for e in range(8)]
    cap = max(512, int(-(-max(len(t) for t in toks) // 512) * 512))
    maps = []
    for e in range(8):
        he = np.zeros((cap, 2048), ml_dtypes.bfloat16)
        he[:len(toks[e])] = H[toks[e]]
        maps.append(dict(hb=he, wg=inp["l1_e_gate"][e:e + 1], wu=inp["l1_e_up"][e:e + 1], wd=inp["l1_e_down"][e:e + 1]))
    nc = k3_program(cap, 7168, "ffn")
    r7 = _run(nc, maps)
    NT = NCORES * TPC
    slot = np.zeros((NT, 8), np.int64)
    for e in range(8):
        slot[toks[e], e] = np.arange(len(toks[e]))
    order = np.argsort(~sel, axis=1, kind="stable")[:, :2]
    ys, g2 = [], np.take_along_axis(GT, order, axis=1).astype(f32)
    for k in range(2):
        yk = np.zeros((NT, 2048), f32)
        for e in range(8):
            m = order[:, k] == e
            yk[m] = r7[e]["y"][slot[m, e]]
        ys.append(yk)
    nc = combine_program(TPC)
    r6 = _run(nc, [dict(x=r6[c]["y"], y1=ys[0][c * TPC:(c + 1) * TPC], y2=ys[1][c * TPC:(c + 1) * TPC],
                        g2=np.ascontiguousarray(g2[c * TPC:(c + 1) * TPC])) for c in range(NCORES)])
    out = np.concatenate([r6[c]["y"] for c in range(NCORES)], axis=0).reshape(2, SEQ, 2048).astype(f32)
    return out
```

```python
import numpy as np
import concourse.bass as bass
import concourse.mybir as mybir
from concourse.bass_utils import run_bass_kernel_spmd

F32 = mybir.dt.float32
BF16 = mybir.dt.bfloat16
I32 = mybir.dt.int32
ALU = mybir.AluOpType
AF = mybir.ActivationFunctionType
AX = mybir.AxisListType

PE, ACT, DVE, POOL, SP = "pe", "act", "dve", "pool", "sp"
ENGS = (PE, ACT, DVE, POOL, SP)
SAME_ENGINE_SYNC = {PE: False, ACT: True, DVE: True, POOL: True, SP: False}


class Buf:
    def __init__(self, name):
        self.name = name
        self.w = []
        self.r = []
        self.dsem = None
        self.dcnt = 0


class Op:
    __slots__ = ("eng", "fn", "deps", "idx", "signal", "count", "is_dma", "dsem", "dcount", "waits", "inc")

    def __init__(self, eng, fn):
        self.eng = eng
        self.fn = fn
        self.deps = []
        self.idx = -1
        self.signal = False
        self.count = 0
        self.is_dma = False
        self.dsem = None
        self.dcount = 0
        self.waits = []
        self.inc = 16


class Prog:
    def __init__(self, nc):
        self.nc = nc
        self.ops = {e: [] for e in ENGS}
        self.sems = {}
        self.dma_sems = []
        self.old_dma = []
        self.nsem = 0

    def _deps(self, reads, writes, acc=False):
        deps = []
        for b in reads:
            deps.extend(b.w)
        for b in writes:
            deps.extend(b.w)
            deps.extend(b.r)
        return deps

    def _commit(self, op, reads, writes):
        for b in writes:
            b.w = [op]
            b.r = []
        for b in reads:
            if b not in writes:
                key = id(op.dsem) if op.is_dma else op.eng
                b.r = [x for x in b.r if (id(x.dsem) if x.is_dma else x.eng) != key]
                b.r.append(op)

    def op(self, eng, fn, reads=(), writes=()):
        o = Op(eng, fn)
        o.deps = self._deps(reads, writes)
        o.idx = len(self.ops[eng])
        self.ops[eng].append(o)
        self._commit(o, reads, writes)
        return o

    def dma(self, eng, fn, sbuf, reads=(), writes=(), inc=16):
        o = Op(eng, fn)
        o.is_dma = True
        o.deps = self._deps(reads, writes)
        o.idx = len(self.ops[eng])
        o.inc = inc
        if sbuf.dsem is None or sbuf.dcnt + 16 > 30000:
            if sbuf.dsem is not None:
                self.old_dma.append((sbuf.dsem, sbuf.dcnt))
            sbuf.dsem = self.nc.alloc_semaphore(f"d_{sbuf.name}_{self.nsem}")
            self.nsem += 1
            sbuf.dcnt = 0
            if sbuf not in self.dma_sems:
                self.dma_sems.append(sbuf)
        sbuf.dcnt += inc
        o.dsem = sbuf.dsem
        o.dcount = sbuf.dcnt
        self.ops[eng].append(o)
        self._commit(o, reads, writes)
        return o

    def emit(self):
        nc = self.nc
        EPOCH = 30000
        for e in ENGS:
            waited_eng = {f: -1 for f in ENGS}
            waited_dma = {}
            for o in self.ops[e]:
                need_eng = {}
                need_dma = {}
                for d in o.deps:
                    if d.is_dma:
                        k = id(d.dsem)
                        if waited_dma.get(k, 0) >= d.dcount:
                            continue
                        if k not in need_dma or need_dma[k][1] < d.dcount:
                            need_dma[k] = (d.dsem, d.dcount)
                    else:
                        if d.eng == e and not SAME_ENGINE_SYNC[e]:
                            continue
                        if waited_eng[d.eng] >= d.idx:
                            continue
                        if d.eng not in need_eng or need_eng[d.eng].idx < d.idx:
                            need_eng[d.eng] = d
                for f, d in need_eng.items():
                    d.signal = True
                    waited_eng[f] = d.idx
                    o.waits.append(d)
                for k, (s, c) in need_dma.items():
                    waited_dma[k] = c
                    o.waits.append((s, c))
        self._final_waits = []
        for e in ENGS:
            if self.ops[e]:
                last = self.ops[e][-1]
                if not last.is_dma:
                    last.signal = True
                    self._final_waits.append(last)
        for e in ENGS:
            c = 0
            cur = None
            for o in self.ops[e]:
                if o.signal and not o.is_dma:
                    if cur is None or c >= EPOCH:
                        cur = nc.alloc_semaphore(f"eng_{e}_{self.nsem}")
                        self.nsem += 1
                        c = 0
                    c += 1
                    o.count = c
                    o.dsem = cur
        prog = self

        def run(e, eng):
            for o in prog.ops[e]:
                for w in o.waits:
                    if isinstance(w, tuple):
                        eng.wait_ge(w[0], w[1])
                    else:
                        eng.wait_ge(w.dsem, w.count)
                ins = o.fn(eng)
                if o.is_dma:
                    ins.then_inc(o.dsem, o.inc)
                elif o.signal:
                    ins.then_inc(o.dsem, 1)
            if e == SP:
                for last in prog._final_waits:
                    if last.eng != SP:
                        eng.wait_ge(last.dsem, last.count)
                for b in prog.dma_sems:
                    eng.wait_ge(b.dsem, b.dcnt)
                for (sm, ct) in prog.old_dma:
                    eng.wait_ge(sm, ct)

        with nc.Block() as block:
            @block.tensor
            def _(eng):
                run(PE, eng)

            @block.scalar
            def _(eng):
                run(ACT, eng)

            @block.vector
            def _(eng):
                run(DVE, eng)

            @block.gpsimd
            def _(eng):
                run(POOL, eng)

            @block.sync
            def _(eng):
                run(SP, eng)


class Tile:
    def __init__(self, nc, name, shape, dtype, psum=False):
        self.name = name
        if psum:
            self.t = nc.alloc_psum_tensor("ps_" + name, list(shape), dtype)
        else:
            self.t = nc.alloc_sbuf_tensor("sb_" + name, list(shape), dtype)
        self.b = Buf(name)
        self.shape = shape

    def __getitem__(self, idx):
        return self.t[idx]


EPS = 1e-6


class Ring:
    def __init__(self, tiles):
        self.tiles = tiles
        self.i = 0

    def next(self):
        t = self.tiles[self.i % len(self.tiles)]
        self.i += 1
        return t


def mk_ring(nc, name, n, shape, dtype, psum=False):
    return Ring([Tile(nc, f"{name}{i}", shape, dtype, psum=psum) for i in range(n)])


def MM(P, out, lhsT, rhs, start, stop, reads, writes):
    P.op(PE, lambda e: e.matmul(out, lhsT=lhsT, rhs=rhs, start=start, stop=stop), reads, writes)


def build_consts(nc, P):
    identf = Tile(nc, "identf", [128, 128], F32)
    ident = Tile(nc, "ident", [128, 128], BF16)
    ones = Tile(nc, "ones", [128, 128], BF16)
    P.op(POOL, lambda e: e.memset(identf[:], 1.0), writes=[identf.b])
    P.op(POOL, lambda e: e.affine_select(identf[:], identf[:], pattern=[[-1, 128]], compare_op=ALU.is_equal,
                                         fill=0.0, base=0, channel_multiplier=1), reads=[identf.b], writes=[identf.b])
    P.op(DVE, lambda e: e.tensor_copy(ident[:], identf[:]), reads=[identf.b], writes=[ident.b])
    P.op(DVE, lambda e: e.memset(ones[:], 1.0), writes=[ones.b])
    return identf, ident, ones


def norm_transpose(P, nc, C, xt, gB, hb, hT, ptr, nsub):
    junk, ss, rstd, ident = C["junk"], C["ss"], C["rstd"], C["ident"]
    for s in range(nsub):
        P.op(ACT, lambda e, s=s: e.activation(junk[:], xt[:, s, :], AF.Square, accum_out=ss[:, s:s + 1]),
             reads=[xt.b], writes=[junk.b, ss.b])
    P.op(ACT, lambda e: e.activation(rstd[:, 0:nsub], ss[:, 0:nsub], AF.Sqrt, bias=C["epsb"][:, 0:1], scale=1.0 / 2048),
         reads=[ss.b, C["epsb"].b], writes=[rstd.b])
    P.op(DVE, lambda e: e.reciprocal(rstd[:, 0:nsub], rstd[:, 0:nsub]), reads=[rstd.b], writes=[rstd.b])
    for s in range(nsub):
        P.op(DVE, lambda e, s=s: e.scalar_tensor_tensor(out=hb[:, s, :], in0=xt[:, s, :], scalar=rstd[:, s:s + 1],
                                                         in1=gB[:], op0=ALU.mult, op1=ALU.mult),
             reads=[xt.b, rstd.b, gB.b], writes=[hb.b])
    transpose_to_fm(P, C, hb, hT, ptr, nsub)


def transpose_to_fm(P, C, hb, hT, ptr, nsub):
    ident = C["ident"]
    for c in range(16):
        pt = ptr.next()
        for s in range(nsub):
            P.op(PE, lambda e, c=c, s=s, pt=pt: e.transpose(pt[:, s * 128:(s + 1) * 128], hb[:, s, c * 128:(c + 1) * 128], ident[:]),
                 reads=[hb.b, ident.b], writes=[pt.b])
        if c % 2 == 0:
            P.op(ACT, lambda e, c=c, pt=pt: e.copy(hT[:, c, 0:nsub * 128], pt[:, 0:nsub * 128]), reads=[pt.b], writes=[hT.bs[c]])
        else:
            P.op(DVE, lambda e, c=c, pt=pt: e.tensor_copy(hT[:, c, 0:nsub * 128], pt[:, 0:nsub * 128]), reads=[pt.b], writes=[hT.bs[c]])


def k1_program(T, NCOL, chunks, vcols, nq, nk, TT=512):
    nc = bass.Bass("TRN2", target_bir_lowering=False)
    X = nc.dram_tensor("x", [T, 2048], F32, kind="ExternalInput").ap()
    G = nc.dram_tensor("g", [2048], F32, kind="ExternalInput").ap()
    W = nc.dram_tensor("w", [2048, NCOL], F32, kind="ExternalInput").ap()
    GQ = nc.dram_tensor("gq", [128], F32, kind="ExternalInput").ap()
    GK = nc.dram_tensor("gk", [128], F32, kind="ExternalInput").ap()
    has_rope = any(ch[3] for ch in chunks)
    two = any(ch[4] in ("q2", "k2") for ch in chunks)
    if two:
        GQ2 = nc.dram_tensor("gq2", [128], F32, kind="ExternalInput").ap()
        GK2 = nc.dram_tensor("gk2", [128], F32, kind="ExternalInput").ap()
    if has_rope:
        COS = nc.dram_tensor("cosT", [128, T], F32, kind="ExternalInput").ap()
        SIN = nc.dram_tensor("sinT", [128, T], F32, kind="ExternalInput").ap()
        RT = nc.dram_tensor("rt", [128, 128], F32, kind="ExternalInput").ap()
    NV = sum(v[1] for v in vcols)
    QT = nc.dram_tensor("qT", [nq, 128, T], BF16, kind="ExternalOutput").ap()
    KT = nc.dram_tensor("kT", [nk, 128, T], BF16, kind="ExternalOutput").ap()
    V = nc.dram_tensor("v", [T, NV], BF16, kind="ExternalOutput").ap()
    P = Prog(nc)
    identf, ident, ones = build_consts(nc, P)
    C = {"ident": ident, "junk": Tile(nc, "junk", [128, 2048], BF16), "ss": Tile(nc, "ss", [128, 4], F32),
         "rstd": Tile(nc, "rstd", [128, 4], F32), "epsb": Tile(nc, "epsb", [128, 1], F32)}
    P.op(DVE, lambda e: e.memset(C["epsb"][:], EPS), writes=[C["epsb"].b])
    gB = Tile(nc, "gB", [128, 2048], F32)
    P.dma(SP, lambda e: e.dma_start(out=gB[:], in_=G.partition_broadcast(128)), gB.b, writes=[gB.b])
    gains = {}
    glist = [("q", GQ, 128 ** -0.5), ("k", GK, 1.0)]
    if two:
        glist += [("q2", GQ2, 128 ** -0.5), ("k2", GK2, 1.0)]
    for nm, ap, sc in glist:
        t = Tile(nc, "gain_" + nm, [128, 1], F32)
        P.dma(SP, lambda e, t=t, ap=ap: e.dma_start(out=t[:], in_=ap.rearrange("(p o) -> p o", o=1)), t.b, writes=[t.b])
        if sc != 1.0:
            P.op(DVE, lambda e, t=t, sc=sc: e.tensor_scalar(t[:], t[:], sc, None, op0=ALU.mult), reads=[t.b], writes=[t.b])
        gains[nm] = t
    rtf = Tile(nc, "rtf", [128, 128], F32)
    rtb = Tile(nc, "rtb", [128, 128], BF16)
    if has_rope:
        P.dma(SP, lambda e: e.dma_start(out=rtf[:], in_=RT), rtf.b, writes=[rtf.b])
        P.op(DVE, lambda e: e.tensor_copy(rtb[:], rtf[:]), reads=[rtf.b], writes=[rtb.b])

    nsub = TT // 128
    xt = Tile(nc, "xt", [128, nsub, 2048], F32)
    hb = Tile(nc, "hb", [128, nsub, 2048], BF16)
    hT = Tile(nc, "hT", [128, 16, TT], BF16)
    hT.bs = [Buf(f"hT{c}") for c in range(16)]
    cs = mk_ring(nc, "cs", 2, [128, 2, TT], F32)
    wr = mk_ring(nc, "wblk", 3, [128, 16, 512], BF16)
    ptr = mk_ring(nc, "pt", 2, [128, 512], BF16, psum=True)
    psr = mk_ring(nc, "ps", 2, [128, 512], F32, psum=True)
    ssr = mk_ring(nc, "ssb", 2, [128, 512], F32, psum=True)
    rqr = mk_ring(nc, "rq", 2, [128, 512], F32, psum=True)
    sqr = mk_ring(nc, "sq", 2, [128, TT], BF16)
    rsr = mk_ring(nc, "rs", 2, [128, TT], F32)
    qnr = mk_ring(nc, "qn", 2, [128, TT], BF16)
    tar = mk_ring(nc, "ta", 2, [128, TT], F32)
    tbr = mk_ring(nc, "tb", 2, [128, TT], F32)
    outr = mk_ring(nc, "outt", 3, [128, TT], BF16)
    vtr = mk_ring(nc, "vt", 2, [128, nsub, 512], BF16)
    nblk = NCOL // 512
    for tt in range(T // TT):
        t0 = tt * TT
        P.dma(SP, lambda e, t0=t0: e.dma_start(out=xt[:], in_=X[t0:t0 + TT, :].rearrange("(s p) d -> p s d", p=128)),
              xt.b, writes=[xt.b])
        if has_rope:
            cst = cs.next()
            P.dma(SP, lambda e, t0=t0, cst=cst: e.dma_start(out=cst[:, 0, :], in_=COS[:, t0:t0 + TT]), cst.b, writes=[cst.b])
            P.dma(SP, lambda e, t0=t0, cst=cst: e.dma_start(out=cst[:, 1, :], in_=SIN[:, t0:t0 + TT]), cst.b, reads=[cst.b], writes=[cst.b])
        norm_transpose(P, nc, C, xt, gB, hb, hT, ptr, nsub)
        for blk in range(nblk):
            c0 = blk * 512
            wb = wr.next()
            P.dma(POOL, lambda e, wb=wb, c0=c0: e.dma_start(out=wb[:], in_=W[:, c0:c0 + 512].rearrange("(c p) n -> p c n", p=128)),
                  wb.b, writes=[wb.b])
            for (col0, kind, oidx, rope, gname) in [ch for ch in chunks if c0 <= ch[0] < c0 + 512]:
                lc = col0 - c0
                ps = psr.next()
                for c in range(16):
                    MM(P, ps[:, 0:TT], wb[:, c, lc:lc + 128], hT[:, c, :], c == 0, c == 15, [wb.b, hT.bs[c]], [ps.b])
                sq = sqr.next()
                P.op(ACT, lambda e, sq=sq, ps=ps: e.activation(sq[:], ps[:, 0:TT], AF.Square), reads=[ps.b], writes=[sq.b])
                ssb = ssr.next()
                MM(P, ssb[:, 0:TT], ones[:], sq[:], True, True, [ones.b, sq.b], [ssb.b])
                rs = rsr.next()
                P.op(ACT, lambda e, rs=rs, ssb=ssb: e.activation(rs[:], ssb[:, 0:TT], AF.Sqrt, bias=C["epsb"][:, 0:1], scale=1.0 / 128),
                     reads=[ssb.b, C["epsb"].b], writes=[rs.b])
                P.op(DVE, lambda e, rs=rs: e.reciprocal(rs[:], rs[:]), reads=[rs.b], writes=[rs.b])
                gt = gains[gname]
                dst = QT if kind == "q" else KT
                ot = outr.next()
                if not rope:
                    P.op(DVE, lambda e, ot=ot, ps=ps, rs=rs, gt=gt: e.scalar_tensor_tensor(
                        out=ot[:], in0=ps[:, 0:TT], scalar=gt[:, 0:1], in1=rs[:], op0=ALU.mult, op1=ALU.mult),
                        reads=[ps.b, rs.b, gt.b], writes=[ot.b])
                else:
                    qn = qnr.next()
                    P.op(DVE, lambda e, qn=qn, ps=ps, rs=rs, gt=gt: e.scalar_tensor_tensor(
                        out=qn[:], in0=ps[:, 0:TT], scalar=gt[:, 0:1], in1=rs[:], op0=ALU.mult, op1=ALU.mult),
                        reads=[ps.b, rs.b, gt.b], writes=[qn.b])
                    rq = rqr.next()
                    MM(P, rq[:, 0:TT], rtb[:], qn[:], True, True, [rtb.b, qn.b], [rq.b])
                    ta = tar.next()
                    tb = tbr.next()
                    P.op(POOL, lambda e, ta=ta, qn=qn, cst=cst: e.tensor_tensor(ta[:], qn[:], cst[:, 0, :], op=ALU.mult),
                         reads=[qn.b, cst.b], writes=[ta.b])
                    P.op(DVE, lambda e, tb=tb, rq=rq, cst=cst: e.tensor_tensor(tb[:], rq[:, 0:TT], cst[:, 1, :], op=ALU.mult),
                         reads=[rq.b, cst.b], writes=[tb.b])
                    P.op(POOL, lambda e, ot=ot, ta=ta, tb=tb: e.tensor_tensor(ot[:], ta[:], tb[:], op=ALU.add),
                         reads=[ta.b, tb.b], writes=[ot.b])
                P.dma(SP, lambda e, dst=dst, oidx=oidx, t0=t0, ot=ot: e.dma_start(out=dst[oidx, :, t0:t0 + TT], in_=ot[:]),
                      ot.b, reads=[ot.b])
            for (col0, ncols, ocol0) in [v for v in vcols if c0 <= v[0] < c0 + 512]:
                lc = col0 - c0
                vt = vtr.next()
                for s in range(nsub):
                    ps = psr.next()
                    for c in range(16):
                        MM(P, ps[:, 0:ncols], hT[:, c, s * 128:(s + 1) * 128], wb[:, c, lc:lc + ncols], c == 0, c == 15,
                           [wb.b, hT.bs[c]], [ps.b])
                    if s % 2 == 0:
                        P.op(ACT, lambda e, vt=vt, ps=ps, s=s, ncols=ncols: e.copy(vt[:, s, 0:ncols], ps[:, 0:ncols]),
                             reads=[ps.b], writes=[vt.b])
                    else:
                        P.op(DVE, lambda e, vt=vt, ps=ps, s=s, ncols=ncols: e.tensor_copy(vt[:, s, 0:ncols], ps[:, 0:ncols]),
                             reads=[ps.b], writes=[vt.b])
                P.dma(SP, lambda e, vt=vt, t0=t0, ocol0=ocol0, ncols=ncols: e.dma_start(
                    out=V[t0:t0 + TT, ocol0:ocol0 + ncols].rearrange("(s p) n -> p s n", p=128), in_=vt[:, :, 0:ncols]),
                    vt.b, reads=[vt.b])
    P.emit()
    return nc


def l0_spec():
    chunks = []
    for h in range(8):
        chunks.append((h * 128, "q", h, True, "q"))
    for h in range(2):
        chunks.append((1024 + h * 128, "k", h, True, "k"))
    for h in range(8):
        chunks.append((1536 + h * 128, "q", 8 + h, False, "q2"))
    for h in range(8):
        chunks.append((2560 + h * 128, "k", 2 + h, False, "k2"))
    vcols = [(1280, 256, 0), (3584, 512, 256), (4096, 512, 768)]
    return dict(NCOL=4608, chunks=chunks, vcols=vcols, nq=16, nk=10)


def l1_spec():
    chunks = []
    for c in range(16):
        chunks.append((c * 128, "q", c, False, "q"))
    for c in range(16):
        chunks.append((2048 + c * 128, "k", c, False, "k"))
    vcols = [(4096 + i * 512, 512, i * 512) for i in range(4)]
    return dict(NCOL=6144, chunks=chunks, vcols=vcols, nq=16, nk=16)


def rope_tables(pos):
    pos = np.asarray(pos)
    row = (pos // 64).astype(np.float32)
    col = (pos % 64).astype(np.float32)
    nf = 32
    inv = (np.float32(10000.0) ** (-np.arange(nf, dtype=np.float32) / np.float32(nf))).astype(np.float32)
    ar = (row[None, :] * inv[:, None]).astype(np.float32)
    ac = (col[None, :] * inv[:, None]).astype(np.float32)
    ang = np.concatenate([ar, ar, ac, ac], axis=0)
    R = np.zeros((128, 128), np.float32)
    for base in (0, 64):
        for i in range(32):
            R[base + i, base + i + 32] = -1.0
            R[base + 32 + i, base + i] = 1.0
    return np.cos(ang).astype(np.float32), np.sin(ang).astype(np.float32), np.ascontiguousarray(R.T)


LAMBDA_INIT = 0.8 - 0.6 * float(np.exp(-0.3 * 1))


def k2_l0_program(T, S, SL, NJ=None):
    nc = bass.Bass("TRN2", target_bir_lowering=False)
    QT = nc.dram_tensor("qT", [16, 128, T], BF16, kind="ExternalInput").ap()
    KT = nc.dram_tensor("kT", [2, 128, S], BF16, kind="ExternalInput").ap()
    V = nc.dram_tensor("v", [S, 256], BF16, kind="ExternalInput").ap()
    KTL = nc.dram_tensor("kTl", [8, 128, SL], BF16, kind="ExternalInput").ap()
    VL = nc.dram_tensor("vl", [SL, 1024], BF16, kind="ExternalInput").ap()
    KM = nc.dram_tensor("kmask", [128, SL // 128], F32, kind="ExternalInput").ap()
    BB = nc.dram_tensor("biasB", [8, 20, 128, 512], F32, kind="ExternalInput").ap()
    OT = nc.dram_tensor("oT", [16, 128, T], BF16, kind="ExternalOutput").ap()
    P = Prog(nc)
    identf, ident, ones = build_consts(nc, P)
    NKB = S // 128
    NKL = SL // 128
    nj = T // 512 if NJ is None else NJ
    kt = Tile(nc, "kt", [128, max(S, SL)], BF16)
    NKC = 4
    kt.bs = [Buf(f"kt{i}") for i in range(NKC)]
    vt = Tile(nc, "vt", [128, max(NKB, NKL), 128], BF16)
    NVC = 8
    vt.bs = [Buf(f"vt{i}") for i in range(NVC)]
    bias = Tile(nc, "bias", [128, 20, 512], F32)
    bias.bs = [Buf(f"bias{i}") for i in range(4)]
    km = Tile(nc, "km", [128, NKL], F32)
    P.dma(SP, lambda e: e.dma_start(out=km[:], in_=KM), km.b, writes=[km.b])
    qr = mk_ring(nc, "q", 2, [128, 512], BF16)
    tr = mk_ring(nc, "t", 3, [128, 512], F32)
    pr = mk_ring(nc, "p", 4, [128, 512], BF16)
    rlr = mk_ring(nc, "rl", 2, [128, 512], F32)
    outr = mk_ring(nc, "o", 2, [128, 512], BF16)
    st = mk_ring(nc, "st", 2, [128, 512], F32, psum=True)
    otr = mk_ring(nc, "ot", 2, [128, 512], F32, psum=True)
    smr = mk_ring(nc, "sm", 2, [128, 512], F32, psum=True)

    def attend(qchunk, ochunk, kbs, kb_buf, kb_ap, v_buf, v_ap, bias_i=None):
        for j in range(nj):
            t0 = j * 512
            q = qr.next()
            P.dma(SP, lambda e, q=q, t0=t0: e.dma_start(out=q[:], in_=QT[qchunk, :, t0:t0 + 512]), q.b, writes=[q.b])
            ot = otr.next()
            sm = smr.next()
            blocks = kbs(j)
            n = len(blocks)
            pend = None

            def qk(i):
                kb = blocks[i]
                s = st.next()
                MM(P, s[:], kb_ap(kb), q[:], True, True, [kb_buf(kb), q.b], [s.b])
                p = pr.next()
                if bias_i is None:
                    P.op(ACT, lambda e, p=p, s=s: e.activation(p[:], s[:], AF.Exp), reads=[s.b], writes=[p.b])
                else:
                    t = tr.next()
                    bi = bias_i(j, i)
                    P.op(DVE, lambda e, t=t, s=s, bi=bi: e.tensor_tensor(t[:], s[:], bias[:, bi, :], op=ALU.add),
                         reads=[s.b, bias.bs[bi // 5]], writes=[t.b])
                    P.op(ACT, lambda e, p=p, t=t, kb=kb: e.activation(p[:], t[:], AF.Exp, bias=km[:, kb:kb + 1]),
                         reads=[t.b, km.b], writes=[p.b])
                return (i, kb, p)

            pend = qk(0)
            for i in range(n):
                nxt = qk(i + 1) if i + 1 < n else None
                (ii, kb, p) = pend
                MM(P, ot[:], v_ap(kb), p[:], ii == 0, ii == n - 1, [v_buf(kb), p.b], [ot.b])
                MM(P, sm[:], ones[:], p[:], ii == 0, ii == n - 1, [ones.b, p.b], [sm.b])
                pend = nxt
            rl = rlr.next()
            P.op(DVE, lambda e, rl=rl, sm=sm: e.reciprocal(rl[:], sm[:]), reads=[sm.b], writes=[rl.b])
            o = outr.next()
            P.op(DVE, lambda e, o=o, ot=ot, rl=rl: e.tensor_tensor(o[:], ot[:], rl[:], op=ALU.mult), reads=[ot.b, rl.b], writes=[o.b])
            P.dma(SP, lambda e, o=o, t0=t0: e.dma_start(out=OT[ochunk, :, t0:t0 + 512], in_=o[:]), o.b, reads=[o.b])

    for kvh in range(2):
        cw = S // NKC
        for i in range(NKC):
            P.dma(SP, lambda e, i=i, kvh=kvh: e.dma_start(out=kt[:, i * cw:(i + 1) * cw], in_=KT[kvh, :, i * cw:(i + 1) * cw]),
                  kt.bs[i], writes=[kt.bs[i]])
        vb = NKB // NVC
        for i in range(NVC):
            P.dma(SP, lambda e, i=i, kvh=kvh: e.dma_start(
                out=vt[:, i * vb:(i + 1) * vb, :],
                in_=V[i * vb * 128:(i + 1) * vb * 128, kvh * 128:(kvh + 1) * 128].rearrange("(kb p) d -> p kb d", p=128)),
                vt.bs[i], writes=[vt.bs[i]])
        for qh in range(4 * kvh, 4 * kvh + 4):
            attend(qh, qh, lambda j: list(range(NKB)),
                   lambda kb: kt.bs[kb * 128 // cw], lambda kb: kt[:, kb * 128:(kb + 1) * 128],
                   lambda kb: vt.bs[kb // vb], lambda kb: vt[:, kb, :])
    for h in range(8):
        P.dma(SP, lambda e, h=h: e.dma_start(out=kt[:, 0:SL], in_=KTL[h, :, :]), kt.bs[0],
              reads=[b for b in kt.bs], writes=[b for b in kt.bs])
        nvl = 3 if NKL % 3 == 0 else 1
        vlb = NKL // nvl
        for i in range(nvl):
            P.dma(SP, lambda e, h=h, i=i: e.dma_start(out=vt[:, i * vlb:(i + 1) * vlb, :],
                                                 in_=VL[i * vlb * 128:(i + 1) * vlb * 128, h * 128:(h + 1) * 128].rearrange("(kb p) d -> p kb d", p=128)),
                  vt.bs[i], reads=[b for b in vt.bs], writes=[b for b in vt.bs])
        for i in range(4):
            P.dma(SP, lambda e, h=h, i=i: e.dma_start(out=bias[:, i * 5:(i + 1) * 5, :],
                                                      in_=BB[h, i * 5:(i + 1) * 5].rearrange("i p q -> p i q")),
                  bias.bs[i], writes=[bias.bs[i]])
        attend(8 + h, 8 + h, lambda j: [4 * j + i for i in range(20)],
               lambda kb: kt.bs[0], lambda kb: kt[:, kb * 128:(kb + 1) * 128],
               lambda kb: vt.bs[0], lambda kb: vt[:, kb, :], bias_i=lambda j, i: i)
    P.emit()
    return nc


def dil_bias_tables():
    kk = np.arange(128)[:, None]
    qq = np.arange(512)[None, :]
    out = np.zeros((8, 20, 128, 512), np.float32)
    slopes = 2.0 ** (-8.0 * np.arange(1, 9) / 8)
    for i in range(20):
        o = 128 * i - 1024 + kk - qq
        a = np.abs(o)
        c = (a <= 64).astype(np.int32) + ((o % 4 == 0) & (a <= 256)) + ((o % 16 == 0) & (a <= 1024))
        lnc = np.where(c > 0, np.log(np.maximum(c, 1)), -30000.0)
        for h in range(8):
            out[h, i] = (-slopes[h] * a + lnc).astype(np.float32)
    return out


def k2_l1_program(T, S, NJ=None, NH=8, win=None):
    nc = bass.Bass("TRN2", target_bir_lowering=False)
    NKB = S // 128
    nj = T // 512 if NJ is None else NJ
    QT = nc.dram_tensor("qT", [16, 128, T], BF16, kind="ExternalInput").ap()
    KT = nc.dram_tensor("kT", [16, 128, S], BF16, kind="ExternalInput").ap()
    V = nc.dram_tensor("v", [S, 2048], BF16, kind="ExternalInput").ap()
    DTd = nc.dram_tensor("DT", [128, (T // 512) * NKB], F32, kind="ExternalInput").ap()
    KQd = nc.dram_tensor("KQ", [128, 512], F32, kind="ExternalInput").ap()
    LAMd = nc.dram_tensor("lamv", [128, 4], F32, kind="ExternalInput").ap()
    SUBd = nc.dram_tensor("subln", [256], F32, kind="ExternalInput").ap()
    OT = nc.dram_tensor("oT", [16, 128, T], BF16, kind="ExternalOutput").ap()
    P = Prog(nc)
    identf, ident, ones = build_consts(nc, P)
    onesf = Tile(nc, "onesf", [128, 128], F32)
    P.op(POOL, lambda e: e.memset(onesf[:], 1.0), writes=[onesf.b])
    epsb = Tile(nc, "epsb", [128, 1], F32)
    P.op(DVE, lambda e: e.memset(epsb[:], EPS), writes=[epsb.b])
    dt = Tile(nc, "dt", [128, (T // 512) * NKB], F32)
    kq = Tile(nc, "kq", [128, 512], F32)
    lamv = Tile(nc, "lamv", [128, 4], F32)
    sub = Tile(nc, "sub", [128, 2], F32)
    P.dma(SP, lambda e: e.dma_start(out=dt[:], in_=DTd), dt.b, writes=[dt.b])
    P.dma(SP, lambda e: e.dma_start(out=kq[:], in_=KQd), kq.b, writes=[kq.b])
    P.dma(SP, lambda e: e.dma_start(out=lamv[:], in_=LAMd), lamv.b, writes=[lamv.b])
    P.dma(SP, lambda e: e.dma_start(out=sub[:, 0:1], in_=SUBd[0:128].rearrange("(p o) -> p o", o=1)), sub.b, writes=[sub.b])
    P.dma(SP, lambda e: e.dma_start(out=sub[:, 1:2], in_=SUBd[128:256].rearrange("(p o) -> p o", o=1)), sub.b, writes=[sub.b])
    P.op(DVE, lambda e: e.tensor_scalar(sub[:], sub[:], 1.0 - LAMBDA_INIT, None, op0=ALU.mult), reads=[sub.b], writes=[sub.b])
    pr2 = Tile(nc, "pr2", [128, 2], F32)
    neglam = Tile(nc, "neglam", [128, 1], F32)
    st = mk_ring(nc, "st", 2, [128, 512], F32, psum=True)
    P.op(DVE, lambda e: e.tensor_tensor(pr2[:, 0:1], lamv[:, 0:1], lamv[:, 1:2], op=ALU.mult), reads=[lamv.b], writes=[pr2.b])
    P.op(DVE, lambda e: e.tensor_tensor(pr2[:, 1:2], lamv[:, 2:3], lamv[:, 3:4], op=ALU.mult), reads=[lamv.b, pr2.b], writes=[pr2.b])
    s0 = st.next()
    MM(P, s0[:, 0:2], onesf[:], pr2[:], True, True, [onesf.b, pr2.b], [s0.b])
    P.op(ACT, lambda e: e.activation(pr2[:], s0[:, 0:2], AF.Exp), reads=[s0.b], writes=[pr2.b])
    P.op(DVE, lambda e: e.tensor_tensor(neglam[:], pr2[:, 1:2], pr2[:, 0:1], op=ALU.subtract), reads=[pr2.b], writes=[neglam.b])
    P.op(DVE, lambda e: e.tensor_scalar(neglam[:], neglam[:], -LAMBDA_INIT, None, op0=ALU.add), reads=[neglam.b], writes=[neglam.b])

    kt = Tile(nc, "kt", [128, 2, S], BF16)
    NKC = 4
    kt.bs = [[Buf(f"kt{c}_{i}") for i in range(NKC)] for c in range(2)]
    vt = Tile(nc, "vt", [128, NKB, 256], BF16)
    NVC = 8
    vt.bs = [Buf(f"vt{i}") for i in range(NVC)]
    qr = mk_ring(nc, "q", 2, [128, 2, 512], BF16)
    abr = mk_ring(nc, "ab", 3, [128, 512], F32)
    tr = mk_ring(nc, "t", 3, [128, 512], F32)
    pr = mk_ring(nc, "p", 4, [128, 512], BF16)
    ot = [Tile(nc, f"ot{i}", [128, 512], F32, psum=True) for i in range(4)]
    sm = [Tile(nc, f"sm{i}", [128, 512], F32, psum=True) for i in range(2)]
    rl = [Tile(nc, f"rl{i}", [128, 512], F32) for i in range(2)]
    oc = [Tile(nc, f"oc{i}", [128, 512], F32) for i in range(4)]
    dd = [Tile(nc, f"dd{i}", [128, 512], F32) for i in range(2)]
    sq = [Tile(nc, f"sq{i}", [128, 512], BF16) for i in range(2)]
    rs = Tile(nc, "rs", [128, 512], F32)
    outr = mk_ring(nc, "o", 4, [128, 512], BF16)
    slopes = [2.0 ** (-(h + 1)) for h in range(8)]
    cw = S // NKC
    vb = NKB // NVC
    for h in range(NH):
        for c in range(2):
            for i in range(NKC):
                P.dma(SP, lambda e, i=i, c=c, h=h: e.dma_start(out=kt[:, c, i * cw:(i + 1) * cw], in_=KT[2 * h + c, :, i * cw:(i + 1) * cw]),
                      kt.bs[c][i], writes=[kt.bs[c][i]])
        for i in range(NVC):
            P.dma(SP, lambda e, i=i, h=h: e.dma_start(
                out=vt[:, i * vb:(i + 1) * vb, :],
                in_=V[i * vb * 128:(i + 1) * vb * 128, h * 256:(h + 1) * 256].rearrange("(kb p) d -> p kb d", p=128)),
                vt.bs[i], writes=[vt.bs[i]])
        for j in range(nj):
            t0 = j * 512
            q = qr.next()
            P.dma(SP, lambda e, q=q, t0=t0, h=h: e.dma_start(out=q[:], in_=QT[2 * h:2 * h + 2, :, t0:t0 + 512].rearrange("c p t -> p c t")),
                  q.b, writes=[q.b])

            def qk(kb, c, ab):
                s = st.next()
                MM(P, s[:], kt[:, c, kb * 128:(kb + 1) * 128], q[:, c, :], True, True, [kt.bs[c][kb * 128 // cw], q.b], [s.b])
                t = tr.next()
                P.op(DVE, lambda e, t=t, s=s, ab=ab, h=h: e.scalar_tensor_tensor(out=t[:], in0=ab[:], scalar=-slopes[h], in1=s[:],
                                                                              op0=ALU.mult, op1=ALU.add),
                     reads=[ab.b, s.b], writes=[t.b])
                p = pr.next()
                P.op(ACT, lambda e, p=p, t=t: e.activation(p[:], t[:], AF.Exp), reads=[t.b], writes=[p.b])
                return p

            def mkab(kb):
                ab = abr.next()
                idx = j * NKB + kb
                P.op(ACT, lambda e, ab=ab, idx=idx: e.activation(ab[:], kq[:], AF.Abs, bias=dt[:, idx:idx + 1], scale=1.0),
                     reads=[kq.b, dt.b], writes=[ab.b])
                return ab

            def pv(kb, c, p, first, last):
                MM(P, ot[2 * c][:], vt[:, kb, 0:128], p[:], first, last, [vt.bs[kb // vb], p.b], [ot[2 * c].b])
                MM(P, ot[2 * c + 1][:], vt[:, kb, 128:256], p[:], first, last, [vt.bs[kb // vb], p.b], [ot[2 * c + 1].b])
                MM(P, sm[c][:], ones[:], p[:], first, last, [ones.b, p.b], [sm[c].b])

            if win is None or 2 * win[h] + 4 >= NKB:
                slots = list(range(NKB))
            else:
                slots = [(4 * j + o) % NKB for o in range(-win[h], 4 + win[h])]
            ns = len(slots)
            ab = mkab(slots[0])
            pend = [qk(slots[0], 0, ab), qk(slots[0], 1, ab)]
            for si in range(ns):
                kb = slots[si]
                nxt = [None, None]
                abn = mkab(slots[si + 1]) if si + 1 < ns else None
                for c in range(2):
                    if abn is not None:
                        nxt[c] = qk(slots[si + 1], c, abn)
                    pv(kb, c, pend[c], si == 0, si == ns - 1)
                pend = nxt
            for c in range(2):
                P.op(DVE, lambda e, c=c: e.reciprocal(rl[c][:], sm[c][:]), reads=[sm[c].b], writes=[rl[c].b])
                for hf in range(2):
                    i = 2 * c + hf
                    P.op(DVE, lambda e, i=i, c=c: e.tensor_tensor(oc[i][:], ot[i][:], rl[c][:], op=ALU.mult),
                         reads=[ot[i].b, rl[c].b], writes=[oc[i].b])
            ssb = st.next()
            for hf in range(2):
                P.op(DVE, lambda e, hf=hf: e.scalar_tensor_tensor(out=dd[hf][:], in0=oc[2 + hf][:], scalar=neglam[:, 0:1], in1=oc[hf][:],
                                                                 op0=ALU.mult, op1=ALU.add),
                     reads=[oc[2 + hf].b, oc[hf].b, neglam.b], writes=[dd[hf].b])
                P.op(ACT, lambda e, hf=hf: e.activation(sq[hf][:], dd[hf][:], AF.Square), reads=[dd[hf].b], writes=[sq[hf].b])
                MM(P, ssb[:], ones[:], sq[hf][:], hf == 0, hf == 1, [ones.b, sq[hf].b], [ssb.b])
            P.op(ACT, lambda e, ssb=ssb: e.activation(rs[:], ssb[:], AF.Sqrt, bias=epsb[:, 0:1], scale=1.0 / 256),
                 reads=[ssb.b, epsb.b], writes=[rs.b])
            P.op(DVE, lambda e: e.reciprocal(rs[:], rs[:]), reads=[rs.b], writes=[rs.b])
            for hf in range(2):
                o = outr.next()
                P.op(DVE, lambda e, o=o, hf=hf: e.scalar_tensor_tensor(out=o[:], in0=dd[hf][:], scalar=sub[:, hf:hf + 1], in1=rs[:],
                                                                      op0=ALU.mult, op1=ALU.mult),
                     reads=[dd[hf].b, sub.b, rs.b], writes=[o.b])
                P.dma(SP, lambda e, o=o, t0=t0, hf=hf, h=h: e.dma_start(out=OT[2 * h + hf, :, t0:t0 + 512], in_=o[:]), o.b, reads=[o.b])
    P.emit()
    return nc


def k3_program(T, NF, mode, TT=512):
    nc = bass.Bass("TRN2", target_bir_lowering=False)
    if mode != "ffn":
        X = nc.dram_tensor("x", [T, 2048], F32, kind="ExternalInput").ap()
        OTd = nc.dram_tensor("oT", [16, 128, T], BF16, kind="ExternalInput").ap()
        WO = nc.dram_tensor("wo", [2048, 2048], F32, kind="ExternalInput").ap()
        G = nc.dram_tensor("g", [2048], F32, kind="ExternalInput").ap()
    if mode != "router":
        WG = nc.dram_tensor("wg", [1, 2048, NF], F32, kind="ExternalInput").ap()
        WU = nc.dram_tensor("wu", [1, 2048, NF], F32, kind="ExternalInput").ap()
        WD = nc.dram_tensor("wd", [1, NF, 2048], F32, kind="ExternalInput").ap()
    if mode == "router":
        WR = nc.dram_tensor("wr", [2048, 8], F32, kind="ExternalInput").ap()
        HBo = nc.dram_tensor("hb", [T, 2048], BF16, kind="ExternalOutput").ap()
        GTo = nc.dram_tensor("gate", [T, 8], F32, kind="ExternalOutput").ap()
    if mode == "ffn":
        HBi = nc.dram_tensor("hb", [T, 2048], BF16, kind="ExternalInput").ap()
    Y = nc.dram_tensor("y", [T, 2048], F32, kind="ExternalOutput").ap()
    P = Prog(nc)
    identf, ident, ones = build_consts(nc, P)
    C = {"ident": ident, "junk": Tile(nc, "junk", [128, 2048], BF16), "ss": Tile(nc, "ss", [128, 4], F32),
         "rstd": Tile(nc, "rstd", [128, 4], F32), "epsb": Tile(nc, "epsb", [128, 1], F32)}
    P.op(DVE, lambda e: e.memset(C["epsb"][:], EPS), writes=[C["epsb"].b])
    nsub = TT // 128
    NFC = NF // 128
    nb = NFC // 4
    hb = Tile(nc, "hb", [128, nsub, 2048], BF16)
    hT = Tile(nc, "hT", [128, 16, TT], BF16)
    hT.bs = [Buf(f"hT{c}") for c in range(16)]
    wr_ = mk_ring(nc, "wblk", 3, [128, 16, 512], BF16)
    ptr = mk_ring(nc, "pt", 2, [128, 512], BF16, psum=True)
    psr = mk_ring(nc, "ps", 4, [128, 512], F32, psum=True)
    xt = Tile(nc, "xt", [128, nsub, 2048], F32)
    if mode != "ffn":
        gB = Tile(nc, "gB", [128, 2048], F32)
        P.dma(SP, lambda e: e.dma_start(out=gB[:], in_=G.partition_broadcast(128)), gB.b, writes=[gB.b])
    if mode != "router":
        A = Tile(nc, "A", [128, NFC, TT], BF16)
        A.bs = [Buf(f"A{c}") for c in range(NFC)]
        sgr = mk_ring(nc, "sg", 3, [128, TT], F32)
    if mode == "router":
        wrt = Tile(nc, "wrt", [128, 16, 8], F32)
        P.dma(SP, lambda e: e.dma_start(out=wrt[:], in_=WR.rearrange("(c p) n -> p c n", p=128)), wrt.b, writes=[wrt.b])
        hf = Tile(nc, "hf", [128, 2048], F32)
        hTf = Tile(nc, "hTf", [128, 16, 128], F32)
        pf = Tile(nc, "pf", [128, 512], F32, psum=True)
        pl = Tile(nc, "pl", [128, 512], F32, psum=True)
        lg = Tile(nc, "lg", [128, 8], F32)
        m8 = Tile(nc, "m8", [128, 8], F32)
        gs = Tile(nc, "gs", [128, 4], F32)
        eq = Tile(nc, "eq", [128, 16], F32)
        gate = Tile(nc, "gate", [128, nsub, 8], F32)

    def wload(src_ap, nchunk):
        wb = wr_.next()
        P.dma(POOL, lambda e, wb=wb: e.dma_start(out=wb[:, 0:nchunk, :], in_=src_ap), wb.b, writes=[wb.b])
        return wb

    for tt in range(T // TT):
        t0 = tt * TT
        if mode == "ffn":
            P.dma(SP, lambda e, t0=t0: e.dma_start(out=hb[:], in_=HBi[t0:t0 + TT, :].rearrange("(s p) d -> p s d", p=128)),
                  hb.b, writes=[hb.b])
            transpose_to_fm(P, C, hb, hT, ptr, nsub)
        else:
            P.dma(SP, lambda e, t0=t0: e.dma_start(out=xt[:], in_=X[t0:t0 + TT, :].rearrange("(s p) d -> p s d", p=128)),
                  xt.b, writes=[xt.b])
            P.dma(SP, lambda e, t0=t0: e.dma_start(out=hT[:], in_=OTd[:, :, t0:t0 + TT].rearrange("c p t -> p c t")),
                  hT.bs[0], writes=list(hT.bs))
            for cg in range(4):
                wb = wload(WO[:, cg * 512:(cg + 1) * 512].rearrange("(c p) n -> p c n", p=128), 16)
                for s in range(nsub):
                    ps = psr.next()
                    for c in range(16):
                        MM(P, ps[:], hT[:, c, s * 128:(s + 1) * 128], wb[:, c, :], c == 0, c == 15, [wb.b, hT.bs[c]], [ps.b])
                    P.op(DVE, lambda e, ps=ps, s=s, cg=cg: e.tensor_tensor(xt[:, s, cg * 512:(cg + 1) * 512], xt[:, s, cg * 512:(cg + 1) * 512],
                                                                         ps[:], op=ALU.add), reads=[ps.b, xt.b], writes=[xt.b])
            norm_transpose(P, nc, C, xt, gB, hb, hT, ptr, nsub)
        if mode == "router":
            for s in range(nsub):
                P.op(DVE, lambda e, s=s: e.scalar_tensor_tensor(out=hf[:], in0=xt[:, s, :], scalar=C["rstd"][:, s:s + 1], in1=gB[:],
                                                             op0=ALU.mult, op1=ALU.mult), reads=[xt.b, C["rstd"].b, gB.b], writes=[hf.b])
                for g4 in range(4):
                    for i in range(4):
                        c = g4 * 4 + i
                        P.op(PE, lambda e, c=c, i=i: e.transpose(pf[:, i * 128:(i + 1) * 128], hf[:, c * 128:(c + 1) * 128], identf[:]),
                             reads=[hf.b, identf.b], writes=[pf.b])
                    P.op(ACT, lambda e, g4=g4: e.copy(hTf[:, g4 * 4:(g4 + 1) * 4, :], pf[:].rearrange("p (a b) -> p a b", a=4)),
                         reads=[pf.b], writes=[hTf.b])
                for c in range(16):
                    MM(P, pl[:, 0:8], hTf[:, c, :], wrt[:, c, :], c == 0, c == 15, [hTf.b, wrt.b], [pl.b])
                P.op(DVE, lambda e: e.tensor_copy(lg[:], pl[:, 0:8]), reads=[pl.b], writes=[lg.b])
                P.op(DVE, lambda e: e.max(m8[:], lg[:]), reads=[lg.b], writes=[m8.b])
                P.op(DVE, lambda e: e.tensor_tensor(gs[:, 0:1], m8[:, 1:2], m8[:, 0:1], op=ALU.subtract), reads=[m8.b], writes=[gs.b])
                P.op(ACT, lambda e: e.activation(gs[:, 0:1], gs[:, 0:1], AF.Exp), reads=[gs.b], writes=[gs.b])
                P.op(DVE, lambda e: e.tensor_scalar(gs[:, 1:2], gs[:, 0:1], 1.0, None, op0=ALU.add), reads=[gs.b], writes=[gs.b])
                P.op(DVE, lambda e: e.reciprocal(gs[:, 1:2], gs[:, 1:2]), reads=[gs.b], writes=[gs.b])
                P.op(DVE, lambda e: e.tensor_tensor(gs[:, 2:3], gs[:, 0:1], gs[:, 1:2], op=ALU.mult), reads=[gs.b], writes=[gs.b])
                P.op(DVE, lambda e: e.tensor_scalar(eq[:, 0:8], lg[:], m8[:, 0:1], gs[:, 1:2], op0=ALU.is_equal, op1=ALU.mult),
                     reads=[lg.b, m8.b, gs.b], writes=[eq.b])
                P.op(DVE, lambda e: e.tensor_scalar(eq[:, 8:16], lg[:], m8[:, 1:2], gs[:, 2:3], op0=ALU.is_equal, op1=ALU.mult),
                     reads=[lg.b, m8.b, gs.b, eq.b], writes=[eq.b])
                P.op(DVE, lambda e, s=s: e.tensor_tensor(gate[:, s, :], eq[:, 0:8], eq[:, 8:16], op=ALU.add), reads=[eq.b], writes=[gate.b])
            P.dma(SP, lambda e, t0=t0: e.dma_start(out=HBo[t0:t0 + TT, :].rearrange("(s p) d -> p s d", p=128), in_=hb[:]),
                  hb.b, reads=[hb.b])
            P.dma(SP, lambda e, t0=t0: e.dma_start(out=GTo[t0:t0 + TT, :].rearrange("(s p) d -> p s d", p=128), in_=gate[:]),
                  gate.b, reads=[gate.b])
        else:
            for fb in range(NF // 512):
                wgb = wload(WG[0, :, fb * 512:(fb + 1) * 512].rearrange("(c p) n -> p c n", p=128), 16)
                wub = wload(WU[0, :, fb * 512:(fb + 1) * 512].rearrange("(c p) n -> p c n", p=128), 16)
                for fc in range(4):
                    pg = psr.next()
                    for c in range(16):
                        MM(P, pg[:, 0:TT], wgb[:, c, fc * 128:(fc + 1) * 128], hT[:, c, :], c == 0, c == 15, [wgb.b, hT.bs[c]], [pg.b])
                    pu = psr.next()
                    for c in range(16):
                        MM(P, pu[:, 0:TT], wub[:, c, fc * 128:(fc + 1) * 128], hT[:, c, :], c == 0, c == 15, [wub.b, hT.bs[c]], [pu.b])
                    sg = sgr.next()
                    P.op(ACT, lambda e, sg=sg, pg=pg: e.activation(sg[:], pg[:, 0:TT], AF.Silu), reads=[pg.b], writes=[sg.b])
                    ac = fb * 4 + fc
                    P.op(DVE, lambda e, sg=sg, pu=pu, ac=ac: e.tensor_tensor(A[:, ac, :], sg[:], pu[:, 0:TT], op=ALU.mult),
                         reads=[sg.b, pu.b], writes=[A.bs[ac]])
            for cg in range(4):
                accs = [psr.next() for _ in range(nsub)]
                for rb in range(4):
                    wdb = wload(WD[0, rb * nb * 128:(rb + 1) * nb * 128, cg * 512:(cg + 1) * 512].rearrange("(c p) n -> p c n", p=128), nb)
                    for s in range(nsub):
                        for c in range(nb):
                            ac = rb * nb + c
                            MM(P, accs[s][:], A[:, ac, s * 128:(s + 1) * 128], wdb[:, c, :], rb == 0 and c == 0, rb == 3 and c == nb - 1,
                               [wdb.b, A.bs[ac]], [accs[s].b])
                for s in range(nsub):
                    acc = accs[s]
                    if mode == "ffn":
                        if s % 2 == 0:
                            P.op(ACT, lambda e, acc=acc, s=s, cg=cg: e.copy(xt[:, s, cg * 512:(cg + 1) * 512], acc[:]),
                                 reads=[acc.b, xt.b], writes=[xt.b])
                        else:
                            P.op(DVE, lambda e, acc=acc, s=s, cg=cg: e.tensor_copy(xt[:, s, cg * 512:(cg + 1) * 512], acc[:]),
                                 reads=[acc.b, xt.b], writes=[xt.b])
                    else:
                        P.op(DVE, lambda e, acc=acc, s=s, cg=cg: e.tensor_tensor(
                            xt[:, s, cg * 512:(cg + 1) * 512], xt[:, s, cg * 512:(cg + 1) * 512], acc[:], op=ALU.add),
                            reads=[acc.b, xt.b], writes=[xt.b])
        P.dma(SP, lambda e, t0=t0: e.dma_start(out=Y[t0:t0 + TT, :].rearrange("(s p) d -> p s d", p=128), in_=xt[:]),
              xt.b, reads=[xt.b])
    P.emit()
    return nc


def combine_program(T, TT=512):
    nc = bass.Bass("TRN2", target_bir_lowering=False)
    X = nc.dram_tensor("x", [T, 2048], F32, kind="ExternalInput").ap()
    Y1 = nc.dram_tensor("y1", [T, 2048], F32, kind="ExternalInput").ap()
    Y2 = nc.dram_tensor("y2", [T, 2048], F32, kind="ExternalInput").ap()
    GG = nc.dram_tensor("g2", [T, 2], F32, kind="ExternalInput").ap()
    Y = nc.dram_tensor("y", [T, 2048], F32, kind="ExternalOutput").ap()
    P = Prog(nc)
    nsub = TT // 128
    xr = mk_ring(nc, "x", 2, [128, nsub, 2048], F32)
    ar = mk_ring(nc, "a", 2, [128, nsub, 2048], F32)
    gr = mk_ring(nc, "g", 2, [128, nsub, 2], F32)
    for tt in range(T // TT):
        t0 = tt * TT
        xt, gt = xr.next(), gr.next()
        P.dma(SP, lambda e, t0=t0, xt=xt: e.dma_start(out=xt[:], in_=X[t0:t0 + TT, :].rearrange("(s p) d -> p s d", p=128)), xt.b, writes=[xt.b])
        P.dma(SP, lambda e, t0=t0, gt=gt: e.dma_start(out=gt[:], in_=GG[t0:t0 + TT, :].rearrange("(s p) d -> p s d", p=128)), gt.b, writes=[gt.b])
        for k, src in enumerate((Y1, Y2)):
            at = ar.next()
            P.dma(SP, lambda e, t0=t0, at=at, src=src: e.dma_start(out=at[:], in_=src[t0:t0 + TT, :].rearrange("(s p) d -> p s d", p=128)),
                  at.b, writes=[at.b])
            for s in range(nsub):
                P.op(DVE, lambda e, at=at, xt=xt, gt=gt, s=s, k=k: e.scalar_tensor_tensor(
                    out=xt[:, s, :], in0=at[:, s, :], scalar=gt[:, s, k:k + 1], in1=xt[:, s, :], op0=ALU.mult, op1=ALU.add),
                    reads=[at.b, gt.b, xt.b], writes=[xt.b])
        P.dma(SP, lambda e, t0=t0, xt=xt: e.dma_start(out=Y[t0:t0 + TT, :].rearrange("(s p) d -> p s d", p=128), in_=xt[:]), xt.b, reads=[xt.b])
    P.emit()
    return nc


import ml_dtypes

NCORES = 8
TPC = 4096
SEQ = 16384
L1_WIN = [min(128, int(np.ceil((155.0 / 2.0 ** (-(h + 1))) / 128.0)) + 1) for h in range(8)]


def _run(nc, in_maps):
    res = run_bass_kernel_spmd(nc, in_maps, core_ids=list(range(NCORES)))
    return res.results


def _gather_batch(rs, key, axis):
    out = []
    for b in range(2):
        out.append(np.concatenate([rs[4 * b + q][key] for q in range(4)], axis=axis))
    return out


def kernel(**inp):
    f32 = np.float32
    x = np.ascontiguousarray(inp["x"], dtype=f32).reshape(NCORES * TPC, 2048)
    xs = [x[c * TPC:(c + 1) * TPC] for c in range(NCORES)]
    tabs = [rope_tables((c % 4) * TPC + np.arange(TPC)) for c in range(4)]
    nc = k1_program(TPC, **l0_spec())
    r1 = _run(nc, [dict(x=xs[c], g=inp["l0_norm_mix"], w=inp["l0_w_in"], gq=inp["l0_qnorm_a"], gk=inp["l0_knorm_a"],
                        gq2=inp["l0_qnorm_b"], gk2=inp["l0_knorm_b"], cosT=tabs[c % 4][0], sinT=tabs[c % 4][1], rt=tabs[c % 4][2])
                   for c in range(NCORES)])
    kT_b = _gather_batch(r1, "kT", 2)
    v_b = _gather_batch(r1, "v", 0)
    biasB = dil_bias_tables()
    SL = TPC + 2048
    maps = []
    for c in range(NCORES):
        b, q = c // 4, c % 4
        lo = q * TPC - 1024
        kTl = np.zeros((8, 128, SL), ml_dtypes.bfloat16)
        vl = np.zeros((SL, 1024), ml_dtypes.bfloat16)
        kmask = np.full((128, SL // 128), -30000.0, f32)
        a0, a1 = max(lo, 0), min(lo + SL, SEQ)
        kTl[:, :, a0 - lo:a1 - lo] = kT_b[b][2:10, :, a0:a1]
        vl[a0 - lo:a1 - lo] = v_b[b][a0:a1, 256:1280]
        kmask[:, (a0 - lo) // 128:(a1 - lo) // 128] = 0.0
        maps.append(dict(qT=r1[c]["qT"], kT=np.ascontiguousarray(kT_b[b][0:2]), v=np.ascontiguousarray(v_b[b][:, 0:256]),
                         kTl=kTl, vl=vl, kmask=kmask, biasB=biasB))
    nc = k2_l0_program(TPC, SEQ, SL)
    r2 = _run(nc, maps)
    nc = k3_program(TPC, 5632, "dense")
    r3 = _run(nc, [dict(x=xs[c], oT=r2[c]["oT"], wo=inp["l0_w_out"], g=inp["l0_norm_ffn"], wg=inp["l0_w_gate"][None],
                        wu=inp["l0_w_up"][None], wd=inp["l0_w_down"][None]) for c in range(NCORES)])
    x1 = [r3[c]["y"] for c in range(NCORES)]
    nc = k1_program(TPC, **l1_spec())
    r4 = _run(nc, [dict(x=x1[c], g=inp["l1_norm_mix"], w=inp["l1_w_in"], gq=inp["l1_qnorm_c"], gk=inp["l1_knorm_c"])
                   for c in range(NCORES)])
    kT_b = _gather_batch(r4, "kT", 2)
    v_b = _gather_batch(r4, "v", 0)
    KQ = (np.arange(128)[:, None] - np.arange(512)[None, :]).astype(f32)
    lamv = np.stack([inp["l1_lambda_q1"], inp["l1_lambda_k1"], inp["l1_lambda_q2"], inp["l1_lambda_k2"]], axis=1).astype(f32)
    maps = []
    for c in range(NCORES):
        b, q = c // 4, c % 4
        DT = np.zeros((128, 8 * 128), f32)
        kbs = (np.arange(128) + 32 * q) % 128
        for j in range(8):
            DT[:, j * 128:(j + 1) * 128] = (kbs * 128 - (q * TPC + j * 512))[None, :]
        maps.append(dict(qT=r4[c]["qT"], kT=np.roll(kT_b[b], -q * TPC, axis=2), v=np.roll(v_b[b], -q * TPC, axis=0),
                         DT=DT, KQ=KQ, lamv=np.ascontiguousarray(lamv), subln=inp["l1_subln"]))
    nc = k2_l1_program(TPC, SEQ, win=L1_WIN)
    r5 = _run(nc, maps)
    nc = k3_program(TPC, 7168, "router")
    r6 = _run(nc, [dict(x=x1[c], oT=r5[c]["oT"], wo=inp["l1_w_out"], g=inp["l1_norm_ffn"], wr=inp["l1_w_router"])
                   for c in range(NCORES)])
    H = np.concatenate([r6[c]["hb"] for c in range(NCORES)], axis=0)
    GT = np.concatenate([r6[c]["gate"] for c in range(NCORES)], axis=0)
    sel = GT != 0
    toks = [np.nonzero(sel[:, e])[0] for e in range(8)]
    cap = max(512, int(-(-max(len(t) for t in toks) // 512) * 512))
    maps = []
    for e in range(8):
        he = np.zeros((cap, 2048), ml_dtypes.bfloat16)
        he[:len(toks[e])] = H[toks[e]]
        maps.append(dict(hb=he, wg=inp["l1_e_gate"][e:e + 1], wu=inp["l1_e_up"][e:e + 1], wd=inp["l1_e_down"][e:e + 1]))
    nc = k3_program(cap, 7168, "ffn")
    r7 = _run(nc, maps)
    NT = NCORES * TPC
    slot = np.zeros((NT, 8), np.int64)
    for e in range(8):
        slot[toks[e], e] = np.arange(len(toks[e]))
    order = np.argsort(~sel, axis=1, kind="stable")[:, :2]
    ys, g2 = [], np.take_along_axis(GT, order, axis=1).astype(f32)
    for k in range(2):
        yk = np.zeros((NT, 2048), f32)
        for e in range(8):
            m = order[:, k] == e
            yk[m] = r7[e]["y"][slot[m, e]]
        ys.append(yk)
    nc = combine_program(TPC)
    r6 = _run(nc, [dict(x=r6[c]["y"], y1=ys[0][c * TPC:(c + 1) * TPC], y2=ys[1][c * TPC:(c + 1) * TPC],
                        g2=np.ascontiguousarray(g2[c * TPC:(c + 1) * TPC])) for c in range(NCORES)])
    out = np.concatenate([r6[c]["y"] for c in range(NCORES)], axis=0).reshape(2, SEQ, 2048).astype(f32)
    return out
```

```python
import numpy as np
import concourse.bass as bass
import concourse.mybir as mybir
from concourse.bass_utils import run_bass_kernel_spmd

F32 = mybir.dt.float32
BF16 = mybir.dt.bfloat16
I32 = mybir.dt.int32
ALU = mybir.AluOpType
AF = mybir.ActivationFunctionType
AX = mybir.AxisListType

PE, ACT, DVE, POOL, SP = "pe", "act", "dve", "pool", "sp"
ENGS = (PE, ACT, DVE, POOL, SP)
SAME_ENGINE_SYNC = {PE: False, ACT: True, DVE: True, POOL: True, SP: False}


class Buf:
    def __init__(self, name):
        self.name = name
        self.w = []
        self.r = []
        self.dsem = None
        self.dcnt = 0


class Op:
    __slots__ = ("eng", "fn", "deps", "idx", "signal", "count", "is_dma", "dsem", "dcount", "waits", "inc")

    def __init__(self, eng, fn):
        self.eng = eng
        self.fn = fn
        self.deps = []
        self.idx = -1
        self.signal = False
        self.count = 0
        self.is_dma = False
        self.dsem = None
        self.dcount = 0
        self.waits = []
        self.inc = 16


class Prog:
    def __init__(self, nc):
        self.nc = nc
        self.ops = {e: [] for e in ENGS}
        self.sems = {}
        self.dma_sems = []
        self.old_dma = []
        self.nsem = 0

    def _deps(self, reads, writes, acc=False):
        deps = []
        for b in reads:
            deps.extend(b.w)
        for b in writes:
            deps.extend(b.w)
            deps.extend(b.r)
        return deps

    def _commit(self, op, reads, writes):
        for b in writes:
            b.w = [op]
            b.r = []
        for b in reads:
            if b not in writes:
                key = id(op.dsem) if op.is_dma else op.eng
                b.r = [x for x in b.r if (id(x.dsem) if x.is_dma else x.eng) != key]
                b.r.append(op)

    def op(self, eng, fn, reads=(), writes=()):
        o = Op(eng, fn)
        o.deps = self._deps(reads, writes)
        o.idx = len(self.ops[eng])
        self.ops[eng].append(o)
        self._commit(o, reads, writes)
        return o

    def dma(self, eng, fn, sbuf, reads=(), writes=(), inc=16):
        o = Op(eng, fn)
        o.is_dma = True
        o.deps = self._deps(reads, writes)
        o.idx = len(self.ops[eng])
        o.inc = inc
        if sbuf.dsem is None or sbuf.dcnt + 16 > 30000:
            if sbuf.dsem is not None:
                self.old_dma.append((sbuf.dsem, sbuf.dcnt))
            sbuf.dsem = self.nc.alloc_semaphore(f"d_{sbuf.name}_{self.nsem}")
            self.nsem += 1
            sbuf.dcnt = 0
            if sbuf not in self.dma_sems:
                self.dma_sems.append(sbuf)
        sbuf.dcnt += inc
        o.dsem = sbuf.dsem
        o.dcount = sbuf.dcnt
        self.ops[eng].append(o)
        self._commit(o, reads, writes)
        return o

    def emit(self):
        nc = self.nc
        EPOCH = 30000
        for e in ENGS:
            waited_eng = {f: -1 for f in ENGS}
            waited_dma = {}
            for o in self.ops[e]:
                need_eng = {}
                need_dma = {}
                for d in o.deps:
                    if d.is_dma:
                        k = id(d.dsem)
                        if waited_dma.get(k, 0) >= d.dcount:
                            continue
                        if k not in need_dma or need_dma[k][1] < d.dcount:
                            need_dma[k] = (d.dsem, d.dcount)
                    else:
                        if d.eng == e and not SAME_ENGINE_SYNC[e]:
                            continue
                        if waited_eng[d.eng] >= d.idx:
                            continue
                        if d.eng not in need_eng or need_eng[d.eng].idx < d.idx:
                            need_eng[d.eng] = d
                for f, d in need_eng.items():
                    d.signal = True
                    waited_eng[f] = d.idx
                    o.waits.append(d)
                for k, (s, c) in need_dma.items():
                    waited_dma[k] = c
                    o.waits.append((s, c))
        self._final_waits = []
        for e in ENGS:
            if self.ops[e]:
                last = self.ops[e][-1]
                if not last.is_dma:
                    last.signal = True
                    self._final_waits.append(last)
        for e in ENGS:
            c = 0
            cur = None
            for o in self.ops[e]:
                if o.signal and not o.is_dma:
                    if cur is None or c >= EPOCH:
                        cur = nc.alloc_semaphore(f"eng_{e}_{self.nsem}")
                        self.nsem += 1
                        c = 0
                    c += 1
                    o.count = c
                    o.dsem = cur
        prog = self

        def run(e, eng):
            for o in prog.ops[e]:
                for w in o.waits:
                    if isinstance(w, tuple):
                        eng.wait_ge(w[0], w[1])
                    else:
                        eng.wait_ge(w.dsem, w.count)
                ins = o.fn(eng)
                if o.is_dma:
                    ins.then_inc(o.dsem, o.inc)
                elif o.signal:
                    ins.then_inc(o.dsem, 1)
            if e == SP:
                for last in prog._final_waits:
                    if last.eng != SP:
                        eng.wait_ge(last.dsem, last.count)
                for b in prog.dma_sems:
                    eng.wait_ge(b.dsem, b.dcnt)
                for (sm, ct) in prog.old_dma:
                    eng.wait_ge(sm, ct)

        with nc.Block() as block:
            @block.tensor
            def _(eng):
                run(PE, eng)

            @block.scalar
            def _(eng):
                run(ACT, eng)

            @block.vector
            def _(eng):
                run(DVE, eng)

            @block.gpsimd
            def _(eng):
                run(POOL, eng)

            @block.sync
            def _(eng):
                run(SP, eng)


class Tile:
    def __init__(self, nc, name, shape, dtype, psum=False):
        self.name = name
        if psum:
            self.t = nc.alloc_psum_tensor("ps_" + name, list(shape), dtype)
        else:
            self.t = nc.alloc_sbuf_tensor("sb_" + name, list(shape), dtype)
        self.b = Buf(name)
        self.shape = shape

    def __getitem__(self, idx):
        return self.t[idx]


EPS = 1e-6


class Ring:
    def __init__(self, tiles):
        self.tiles = tiles
        self.i = 0

    def next(self):
        t = self.tiles[self.i % len(self.tiles)]
        self.i += 1
        return t


def mk_ring(nc, name, n, shape, dtype, psum=False):
    return Ring([Tile(nc, f"{name}{i}", shape, dtype, psum=psum) for i in range(n)])


def MM(P, out, lhsT, rhs, start, stop, reads, writes):
    P.op(PE, lambda e: e.matmul(out, lhsT=lhsT, rhs=rhs, start=start, stop=stop), reads, writes)


def build_consts(nc, P):
    identf = Tile(nc, "identf", [128, 128], F32)
    ident = Tile(nc, "ident", [128, 128], BF16)
    ones = Tile(nc, "ones", [128, 128], BF16)
    P.op(POOL, lambda e: e.memset(identf[:], 1.0), writes=[identf.b])
    P.op(POOL, lambda e: e.affine_select(identf[:], identf[:], pattern=[[-1, 128]], compare_op=ALU.is_equal,
                                         fill=0.0, base=0, channel_multiplier=1), reads=[identf.b], writes=[identf.b])
    P.op(DVE, lambda e: e.tensor_copy(ident[:], identf[:]), reads=[identf.b], writes=[ident.b])
    P.op(DVE, lambda e: e.memset(ones[:], 1.0), writes=[ones.b])
    return identf, ident, ones


def norm_transpose(P, nc, C, xt, gB, hb, hT, ptr, nsub):
    junk, ss, rstd, ident = C["junk"], C["ss"], C["rstd"], C["ident"]
    for s in range(nsub):
        P.op(ACT, lambda e, s=s: e.activation(junk[:], xt[:, s, :], AF.Square, accum_out=ss[:, s:s + 1]),
             reads=[xt.b], writes=[junk.b, ss.b])
    P.op(ACT, lambda e: e.activation(rstd[:, 0:nsub], ss[:, 0:nsub], AF.Sqrt, bias=C["epsb"][:, 0:1], scale=1.0 / 2048),
         reads=[ss.b, C["epsb"].b], writes=[rstd.b])
    P.op(DVE, lambda e: e.reciprocal(rstd[:, 0:nsub], rstd[:, 0:nsub]), reads=[rstd.b], writes=[rstd.b])
    for s in range(nsub):
        P.op(DVE, lambda e, s=s: e.scalar_tensor_tensor(out=hb[:, s, :], in0=xt[:, s, :], scalar=rstd[:, s:s + 1],
                                                         in1=gB[:], op0=ALU.mult, op1=ALU.mult),
             reads=[xt.b, rstd.b, gB.b], writes=[hb.b])
    transpose_to_fm(P, C, hb, hT, ptr, nsub)


def transpose_to_fm(P, C, hb, hT, ptr, nsub):
    ident = C["ident"]
    for c in range(16):
        pt = ptr.next()
        for s in range(nsub):
            P.op(PE, lambda e, c=c, s=s, pt=pt: e.transpose(pt[:, s * 128:(s + 1) * 128], hb[:, s, c * 128:(c + 1) * 128], ident[:]),
                 reads=[hb.b, ident.b], writes=[pt.b])
        if c % 2 == 0:
            P.op(ACT, lambda e, c=c, pt=pt: e.copy(hT[:, c, 0:nsub * 128], pt[:, 0:nsub * 128]), reads=[pt.b], writes=[hT.bs[c]])
        else:
            P.op(DVE, lambda e, c=c, pt=pt: e.tensor_copy(hT[:, c, 0:nsub * 128], pt[:, 0:nsub * 128]), reads=[pt.b], writes=[hT.bs[c]])


def k1_program(T, NCOL, chunks, vcols, nq, nk, TT=512):
    nc = bass.Bass("TRN2", target_bir_lowering=False)
    X = nc.dram_tensor("x", [T, 2048], F32, kind="ExternalInput").ap()
    G = nc.dram_tensor("g", [2048], F32, kind="ExternalInput").ap()
    W = nc.dram_tensor("w", [2048, NCOL], F32, kind="ExternalInput").ap()
    GQ = nc.dram_tensor("gq", [128], F32, kind="ExternalInput").ap()
    GK = nc.dram_tensor("gk", [128], F32, kind="ExternalInput").ap()
    has_rope = any(ch[3] for ch in chunks)
    two = any(ch[4] in ("q2", "k2") for ch in chunks)
    if two:
        GQ2 = nc.dram_tensor("gq2", [128], F32, kind="ExternalInput").ap()
        GK2 = nc.dram_tensor("gk2", [128], F32, kind="ExternalInput").ap()
    if has_rope:
        COS = nc.dram_tensor("cosT", [128, T], F32, kind="ExternalInput").ap()
        SIN = nc.dram_tensor("sinT", [128, T], F32, kind="ExternalInput").ap()
        RT = nc.dram_tensor("rt", [128, 128], F32, kind="ExternalInput").ap()
    NV = sum(v[1] for v in vcols)
    QT = nc.dram_tensor("qT", [nq, 128, T], BF16, kind="ExternalOutput").ap()
    KT = nc.dram_tensor("kT", [nk, 128, T], BF16, kind="ExternalOutput").ap()
    V = nc.dram_tensor("v", [T, NV], BF16, kind="ExternalOutput").ap()
    P = Prog(nc)
    identf, ident, ones = build_consts(nc, P)
    C = {"ident": ident, "junk": Tile(nc, "junk", [128, 2048], BF16), "ss": Tile(nc, "ss", [128, 4], F32),
         "rstd": Tile(nc, "rstd", [128, 4], F32), "epsb": Tile(nc, "epsb", [128, 1], F32)}
    P.op(DVE, lambda e: e.memset(C["epsb"][:], EPS), writes=[C["epsb"].b])
    gB = Tile(nc, "gB", [128, 2048], F32)
    P.dma(SP, lambda e: e.dma_start(out=gB[:], in_=G.partition_broadcast(128)), gB.b, writes=[gB.b])
    gains = {}
    glist = [("q", GQ, 128 ** -0.5), ("k", GK, 1.0)]
    if two:
        glist += [("q2", GQ2, 128 ** -0.5), ("k2", GK2, 1.0)]
    for nm, ap, sc in glist:
        t = Tile(nc, "gain_" + nm, [128, 1], F32)
        P.dma(SP, lambda e, t=t, ap=ap: e.dma_start(out=t[:], in_=ap.rearrange("(p o) -> p o", o=1)), t.b, writes=[t.b])
        if sc != 1.0:
            P.op(DVE, lambda e, t=t, sc=sc: e.tensor_scalar(t[:], t[:], sc, None, op0=ALU.mult), reads=[t.b], writes=[t.b])
        gains[nm] = t
    rtf = Tile(nc, "rtf", [128, 128], F32)
    rtb = Tile(nc, "rtb", [128, 128], BF16)
    if has_rope:
        P.dma(SP, lambda e: e.dma_start(out=rtf[:], in_=RT), rtf.b, writes=[rtf.b])
        P.op(DVE, lambda e: e.tensor_copy(rtb[:], rtf[:]), reads=[rtf.b], writes=[rtb.b])

    nsub = TT // 128
    xt = Tile(nc, "xt", [128, nsub, 2048], F32)
    hb = Tile(nc, "hb", [128, nsub, 2048], BF16)
    hT = Tile(nc, "hT", [128, 16, TT], BF16)
    hT.bs = [Buf(f"hT{c}") for c in range(16)]
    cs = mk_ring(nc, "cs", 2, [128, 2, TT], F32)
    wr = mk_ring(nc, "wblk", 3, [128, 16, 512], BF16)
    ptr = mk_ring(nc, "pt", 2, [128, 512], BF16, psum=True)
    psr = mk_ring(nc, "ps", 2, [128, 512], F32, psum=True)
    ssr = mk_ring(nc, "ssb", 2, [128, 512], F32, psum=True)
    rqr = mk_ring(nc, "rq", 2, [128, 512], F32, psum=True)
    sqr = mk_ring(nc, "sq", 2, [128, TT], BF16)
    rsr = mk_ring(nc, "rs", 2, [128, TT], F32)
    qnr = mk_ring(nc, "qn", 2, [128, TT], BF16)
    tar = mk_ring(nc, "ta", 2, [128, TT], F32)
    tbr = mk_ring(nc, "tb", 2, [128, TT], F32)
    outr = mk_ring(nc, "outt", 3, [128, TT], BF16)
    vtr = mk_ring(nc, "vt", 2, [128, nsub, 512], BF16)
    nblk = NCOL // 512
    for tt in range(T // TT):
        t0 = tt * TT
        P.dma(SP, lambda e, t0=t0: e.dma_start(out=xt[:], in_=X[t0:t0 + TT, :].rearrange("(s p) d -> p s d", p=128)),
              xt.b, writes=[xt.b])
        if has_rope:
            cst = cs.next()
            P.dma(SP, lambda e, t0=t0, cst=cst: e.dma_start(out=cst[:, 0, :], in_=COS[:, t0:t0 + TT]), cst.b, writes=[cst.b])
            P.dma(SP, lambda e, t0=t0, cst=cst: e.dma_start(out=cst[:, 1, :], in_=SIN[:, t0:t0 + TT]), cst.b, reads=[cst.b], writes=[cst.b])
        norm_transpose(P, nc, C, xt, gB, hb, hT, ptr, nsub)
        for blk in range(nblk):
            c0 = blk * 512
            wb = wr.next()
            P.dma(POOL, lambda e, wb=wb, c0=c0: e.dma_start(out=wb[:], in_=W[:, c0:c0 + 512].rearrange("(c p) n -> p c n", p=128)),
                  wb.b, writes=[wb.b])
            for (col0, kind, oidx, rope, gname) in [ch for ch in chunks if c0 <= ch[0] < c0 + 512]:
                lc = col0 - c0
                ps = psr.next()
                for c in range(16):
                    MM(P, ps[:, 0:TT], wb[:, c, lc:lc + 128], hT[:, c, :], c == 0, c == 15, [wb.b, hT.bs[c]], [ps.b])
                sq = sqr.next()
                P.op(ACT, lambda e, sq=sq, ps=ps: e.activation(sq[:], ps[:, 0:TT], AF.Square), reads=[ps.b], writes=[sq.b])
                ssb = ssr.next()
                MM(P, ssb[:, 0:TT], ones[:], sq[:], True, True, [ones.b, sq.b], [ssb.b])
                rs = rsr.next()
                P.op(ACT, lambda e, rs=rs, ssb=ssb: e.activation(rs[:], ssb[:, 0:TT], AF.Sqrt, bias=C["epsb"][:, 0:1], scale=1.0 / 128),
                     reads=[ssb.b, C["epsb"].b], writes=[rs.b])
                P.op(DVE, lambda e, rs=rs: e.reciprocal(rs[:], rs[:]), reads=[rs.b], writes=[rs.b])
                gt = gains[gname]
                dst = QT if kind == "q" else KT
                ot = outr.next()
                if not rope:
                    P.op(DVE, lambda e, ot=ot, ps=ps, rs=rs, gt=gt: e.scalar_tensor_tensor(
                        out=ot[:], in0=ps[:, 0:TT], scalar=gt[:, 0:1], in1=rs[:], op0=ALU.mult, op1=ALU.mult),
                        reads=[ps.b, rs.b, gt.b], writes=[ot.b])
                else:
                    qn = qnr.next()
                    P.op(DVE, lambda e, qn=qn, ps=ps, rs=rs, gt=gt: e.scalar_tensor_tensor(
                        out=qn[:], in0=ps[:, 0:TT], scalar=gt[:, 0:1], in1=rs[:], op0=ALU.mult, op1=ALU.mult),
                        reads=[ps.b, rs.b, gt.b], writes=[qn.b])
                    rq = rqr.next()
                    MM(P, rq[:, 0:TT], rtb[:], qn[:], True, True, [rtb.b, qn.b], [rq.b])
                    ta = tar.next()
                    tb = tbr.next()
                    P.op(POOL, lambda e, ta=ta, qn=qn, cst=cst: e.tensor_tensor(ta[:], qn[:], cst[:, 0, :], op=ALU.mult),
                         reads=[qn.b, cst.b], writes=[ta.b])
                    P.op(DVE, lambda e, tb=tb, rq=rq, cst=cst: e.tensor_tensor(tb[:], rq[:, 0:TT], cst[:, 1, :], op=ALU.mult),
                         reads=[rq.b, cst.b], writes=[tb.b])
                    P.op(POOL, lambda e, ot=ot, ta=ta, tb=tb: e.tensor_tensor(ot[:], ta[:], tb[:], op=ALU.add),
                         reads=[ta.b, tb.b], writes=[ot.b])
                P.dma(SP, lambda e, dst=dst, oidx=oidx, t0=t0, ot=ot: e.dma_start(out=dst[oidx, :, t0:t0 + TT], in_=ot[:]),
                      ot.b, reads=[ot.b])
            for (col0, ncols, ocol0) in [v for v in vcols if c0 <= v[0] < c0 + 512]:
                lc = col0 - c0
                vt = vtr.next()
                for s in range(nsub):
                    ps = psr.next()
                    for c in range(16):
                        MM(P, ps[:, 0:ncols], hT[:, c, s * 128:(s + 1) * 128], wb[:, c, lc:lc + ncols], c == 0, c == 15,
                           [wb.b, hT.bs[c]], [ps.b])
                    if s % 2 == 0:
                        P.op(ACT, lambda e, vt=vt, ps=ps, s=s, ncols=ncols: e.copy(vt[:, s, 0:ncols], ps[:, 0:ncols]),
                             reads=[ps.b], writes=[vt.b])
                    else:
                        P.op(DVE, lambda e, vt=vt, ps=ps, s=s, ncols=ncols: e.tensor_copy(vt[:, s, 0:ncols], ps[:, 0:ncols]),
                             reads=[ps.b], writes=[vt.b])
                P.dma(SP, lambda e, vt=vt, t0=t0, ocol0=ocol0, ncols=ncols: e.dma_start(
                    out=V[t0:t0 + TT, ocol0:ocol0 + ncols].rearrange("(s p) n -> p s n", p=128), in_=vt[:, :, 0:ncols]),
                    vt.b, reads=[vt.b])
    P.emit()
    return nc


def l0_spec():
    chunks = []
    for h in range(8):
        chunks.append((h * 128, "q", h, True, "q"))
    for h in range(2):
        chunks.append((1024 + h * 128, "k", h, True, "k"))
    for h in range(8):
        chunks.append((1536 + h * 128, "q", 8 + h, False, "q2"))
    for h in range(8):
        chunks.append((2560 + h * 128, "k", 2 + h, False, "k2"))
    vcols = [(1280, 256, 0), (3584, 512, 256), (4096, 512, 768)]
    return dict(NCOL=4608, chunks=chunks, vcols=vcols, nq=16, nk=10)


def l1_spec():
    chunks = []
    for c in range(16):
        chunks.append((c * 128, "q", c, False, "q"))
    for c in range(16):
        chunks.append((2048 + c * 128, "k", c, False, "k"))
    vcols = [(4096 + i * 512, 512, i * 512) for i in range(4)]
    return dict(NCOL=6144, chunks=chunks, vcols=vcols, nq=16, nk=16)


def rope_tables(pos):
    pos = np.asarray(pos)
    row = (pos // 64).astype(np.float32)
    col = (pos % 64).astype(np.float32)
    nf = 32
    inv = (np.float32(10000.0) ** (-np.arange(nf, dtype=np.float32) / np.float32(nf))).astype(np.float32)
    ar = (row[None, :] * inv[:, None]).astype(np.float32)
    ac = (col[None, :] * inv[:, None]).astype(np.float32)
    ang = np.concatenate([ar, ar, ac, ac], axis=0)
    R = np.zeros((128, 128), np.float32)
    for base in (0, 64):
        for i in range(32):
            R[base + i, base + i + 32] = -1.0
            R[base + 32 + i, base + i] = 1.0
    return np.cos(ang).astype(np.float32), np.sin(ang).astype(np.float32), np.ascontiguousarray(R.T)


LAMBDA_INIT = 0.8 - 0.6 * float(np.exp(-0.3 * 1))


def k2_l0_program(T, S, SL, NJ=None):
    nc = bass.Bass("TRN2", target_bir_lowering=False)
    QT = nc.dram_tensor("qT", [16, 128, T], BF16, kind="ExternalInput").ap()
    KT = nc.dram_tensor("kT", [2, 128, S], BF16, kind="ExternalInput").ap()
    V = nc.dram_tensor("v", [S, 256], BF16, kind="ExternalInput").ap()
    KTL = nc.dram_tensor("kTl", [8, 128, SL], BF16, kind="ExternalInput").ap()
    VL = nc.dram_tensor("vl", [SL, 1024], BF16, kind="ExternalInput").ap()
    KM = nc.dram_tensor("kmask", [128, SL // 128], F32, kind="ExternalInput").ap()
    BB = nc.dram_tensor("biasB", [8, 20, 128, 512], F32, kind="ExternalInput").ap()
    OT = nc.dram_tensor("oT", [16, 128, T], BF16, kind="ExternalOutput").ap()
    P = Prog(nc)
    identf, ident, ones = build_consts(nc, P)
    NKB = S // 128
    NKL = SL // 128
    nj = T // 512 if NJ is None else NJ
    kt = Tile(nc, "kt", [128, max(S, SL)], BF16)
    NKC = 4
    kt.bs = [Buf(f"kt{i}") for i in range(NKC)]
    vt = Tile(nc, "vt", [128, max(NKB, NKL), 128], BF16)
    NVC = 8
    vt.bs = [Buf(f"vt{i}") for i in range(NVC)]
    bias = Tile(nc, "bias", [128, 20, 512], F32)
    bias.bs = [Buf(f"bias{i}") for i in range(4)]
    km = Tile(nc, "km", [128, NKL], F32)
    P.dma(SP, lambda e: e.dma_start(out=km[:], in_=KM), km.b, writes=[km.b])
    qr = mk_ring(nc, "q", 2, [128, 512], BF16)
    tr = mk_ring(nc, "t", 3, [128, 512], F32)
    pr = mk_ring(nc, "p", 4, [128, 512], BF16)
    rlr = mk_ring(nc, "rl", 2, [128, 512], F32)
    outr = mk_ring(nc, "o", 2, [128, 512], BF16)
    st = mk_ring(nc, "st", 2, [128, 512], F32, psum=True)
    otr = mk_ring(nc, "ot", 2, [128, 512], F32, psum=True)
    smr = mk_ring(nc, "sm", 2, [128, 512], F32, psum=True)
    accDr = mk_ring(nc, "accD", 2, [128, 512], F32)
    accPr = mk_ring(nc, "accP", 2, [128, 512], F32)
    onesf = Tile(nc, "onesf", [128, 128], F32)
    P.op(POOL, lambda e: e.memset(onesf[:], 1.0), writes=[onesf.b])

    def attend(qchunk, ochunk, kbs, kb_buf, kb_ap, v_buf, v_ap, bias_i=None):
        for j in range(nj):
            t0 = j * 512
            q = qr.next()
            P.dma(SP, lambda e, q=q, t0=t0: e.dma_start(out=q[:], in_=QT[qchunk, :, t0:t0 + 512]), q.b, writes=[q.b])
            ot = otr.next()
            sm = smr.next()
            accD, accP = accDr.next(), accPr.next()
            blocks = kbs(j)
            n = len(blocks)
            pend = None

            def qk(i):
                kb = blocks[i]
                s = st.next()
                MM(P, s[:], kb_ap(kb), q[:], True, True, [kb_buf(kb), q.b], [s.b])
                p = pr.next()
                if bias_i is None:
                    P.op(ACT, lambda e, p=p, s=s: e.activation(p[:], s[:], AF.Exp), reads=[s.b], writes=[p.b])
                else:
                    t = tr.next()
                    bi = bias_i(j, i)
                    P.op(DVE, lambda e, t=t, s=s, bi=bi: e.tensor_tensor(t[:], s[:], bias[:, bi, :], op=ALU.add),
                         reads=[s.b, bias.bs[bi // 5]], writes=[t.b])
                    P.op(ACT, lambda e, p=p, t=t, kb=kb: e.activation(p[:], t[:], AF.Exp, bias=km[:, kb:kb + 1]),
                         reads=[t.b, km.b], writes=[p.b])
                return (i, kb, p)

            pend = qk(0)
            for i in range(n):
                nxt = qk(i + 1) if i + 1 < n else None
                (ii, kb, p) = pend
                MM(P, ot[:], v_ap(kb), p[:], ii == 0, ii == n - 1, [v_buf(kb), p.b], [ot.b])
                if bias_i is not None:
                    MM(P, sm[:], ones[:], p[:], ii == 0, ii == n - 1, [ones.b, p.b], [sm.b])
                else:
                    eng, acc = (DVE, accD) if ii % 2 == 0 else (POOL, accP)
                    if ii < 2:
                        P.op(eng, lambda e, acc=acc, p=p: e.tensor_copy(acc[:], p[:]), reads=[p.b], writes=[acc.b])
                    else:
                        P.op(eng, lambda e, acc=acc, p=p: e.tensor_tensor(acc[:], acc[:], p[:], op=ALU.add), reads=[p.b, acc.b], writes=[acc.b])
                pend = nxt
            if bias_i is None:
                MM(P, sm[:], onesf[:], accD[:], True, False, [onesf.b, accD.b], [sm.b])
                MM(P, sm[:], onesf[:], accP[:], False, True, [onesf.b, accP.b], [sm.b])
            rl = rlr.next()
            P.op(DVE, lambda e, rl=rl, sm=sm: e.reciprocal(rl[:], sm[:]), reads=[sm.b], writes=[rl.b])
            o = outr.next()
            P.op(DVE, lambda e, o=o, ot=ot, rl=rl: e.tensor_tensor(o[:], ot[:], rl[:], op=ALU.mult), reads=[ot.b, rl.b], writes=[o.b])
            P.dma(SP, lambda e, o=o, t0=t0: e.dma_start(out=OT[ochunk, :, t0:t0 + 512], in_=o[:]), o.b, reads=[o.b])

    for kvh in range(2):
        cw = S // NKC
        for i in range(NKC):
            P.dma(SP, lambda e, i=i, kvh=kvh: e.dma_start(out=kt[:, i * cw:(i + 1) * cw], in_=KT[kvh, :, i * cw:(i + 1) * cw]),
                  kt.bs[i], writes=[kt.bs[i]])
        vb = NKB // NVC
        for i in range(NVC):
            P.dma(SP, lambda e, i=i, kvh=kvh: e.dma_start(
                out=vt[:, i * vb:(i + 1) * vb, :],
                in_=V[i * vb * 128:(i + 1) * vb * 128, kvh * 128:(kvh + 1) * 128].rearrange("(kb p) d -> p kb d", p=128)),
                vt.bs[i], writes=[vt.bs[i]])
        for qh in range(4 * kvh, 4 * kvh + 4):
            attend(qh, qh, lambda j: list(range(NKB)),
                   lambda kb: kt.bs[kb * 128 // cw], lambda kb: kt[:, kb * 128:(kb + 1) * 128],
                   lambda kb: vt.bs[kb // vb], lambda kb: vt[:, kb, :])
    for h in range(8):
        P.dma(SP, lambda e, h=h: e.dma_start(out=kt[:, 0:SL], in_=KTL[h, :, :]), kt.bs[0],
              reads=[b for b in kt.bs], writes=[b for b in kt.bs])
        nvl = 3 if NKL % 3 == 0 else 1
        vlb = NKL // nvl
        for i in range(nvl):
            P.dma(SP, lambda e, h=h, i=i: e.dma_start(out=vt[:, i * vlb:(i + 1) * vlb, :],
                                                 in_=VL[i * vlb * 128:(i + 1) * vlb * 128, h * 128:(h + 1) * 128].rearrange("(kb p) d -> p kb d", p=128)),
                  vt.bs[i], reads=[b for b in vt.bs], writes=[b for b in vt.bs])
        for i in range(4):
            P.dma(SP, lambda e, h=h, i=i: e.dma_start(out=bias[:, i * 5:(i + 1) * 5, :],
                                                      in_=BB[h, i * 5:(i + 1) * 5].rearrange("i p q -> p i q")),
                  bias.bs[i], writes=[bias.bs[i]])
        attend(8 + h, 8 + h, lambda j: [4 * j + i for i in range(20)],
               lambda kb: kt.bs[0], lambda kb: kt[:, kb * 128:(kb + 1) * 128],
               lambda kb: vt.bs[0], lambda kb: vt[:, kb, :], bias_i=lambda j, i: i)
    P.emit()
    return nc


def dil_bias_tables():
    kk = np.arange(128)[:, None]
    qq = np.arange(512)[None, :]
    out = np.zeros((8, 20, 128, 512), np.float32)
    slopes = 2.0 ** (-8.0 * np.arange(1, 9) / 8)
    for i in range(20):
        o = 128 * i - 1024 + kk - qq
        a = np.abs(o)
        c = (a <= 64).astype(np.int32) + ((o % 4 == 0) & (a <= 256)) + ((o % 16 == 0) & (a <= 1024))
        lnc = np.where(c > 0, np.log(np.maximum(c, 1)), -30000.0)
        for h in range(8):
            out[h, i] = (-slopes[h] * a + lnc).astype(np.float32)
    return out


def k2_l1_program(T, S, NJ=None, NH=8, win=None):
    nc = bass.Bass("TRN2", target_bir_lowering=False)
    NKB = S // 128
    nj = T // 512 if NJ is None else NJ
    QT = nc.dram_tensor("qT", [16, 128, T], BF16, kind="ExternalInput").ap()
    KT = nc.dram_tensor("kT", [16, 128, S], BF16, kind="ExternalInput").ap()
    V = nc.dram_tensor("v", [S, 2048], BF16, kind="ExternalInput").ap()
    DTd = nc.dram_tensor("DT", [128, 2, (T // 512) * NKB], F32, kind="ExternalInput").ap()
    KQd = nc.dram_tensor("KQ", [128, 5, 512], F32, kind="ExternalInput").ap()
    LAMd = nc.dram_tensor("lamv", [128, 4], F32, kind="ExternalInput").ap()
    SUBd = nc.dram_tensor("subln", [256], F32, kind="ExternalInput").ap()
    OT = nc.dram_tensor("oT", [16, 128, T], BF16, kind="ExternalOutput").ap()
    P = Prog(nc)
    identf, ident, ones = build_consts(nc, P)
    onesf = Tile(nc, "onesf", [128, 128], F32)
    P.op(POOL, lambda e: e.memset(onesf[:], 1.0), writes=[onesf.b])
    epsb = Tile(nc, "epsb", [128, 1], F32)
    P.op(DVE, lambda e: e.memset(epsb[:], EPS), writes=[epsb.b])
    dt = Tile(nc, "dt", [128, 2, (T // 512) * NKB], F32)
    kq = Tile(nc, "kq", [128, 5, 512], F32)
    qqr = mk_ring(nc, "qqs", 2, [128, 512], F32)
    rbr = mk_ring(nc, "rbh", 2, [128, (T // 512) * NKB], F32)
    lamv = Tile(nc, "lamv", [128, 4], F32)
    sub = Tile(nc, "sub", [128, 2], F32)
    P.dma(SP, lambda e: e.dma_start(out=dt[:], in_=DTd), dt.b, writes=[dt.b])
    P.dma(SP, lambda e: e.dma_start(out=kq[:], in_=KQd), kq.b, writes=[kq.b])
    P.dma(SP, lambda e: e.dma_start(out=lamv[:], in_=LAMd), lamv.b, writes=[lamv.b])
    P.dma(SP, lambda e: e.dma_start(out=sub[:, 0:1], in_=SUBd[0:128].rearrange("(p o) -> p o", o=1)), sub.b, writes=[sub.b])
    P.dma(SP, lambda e: e.dma_start(out=sub[:, 1:2], in_=SUBd[128:256].rearrange("(p o) -> p o", o=1)), sub.b, writes=[sub.b])
    P.op(DVE, lambda e: e.tensor_scalar(sub[:], sub[:], 1.0 - LAMBDA_INIT, None, op0=ALU.mult), reads=[sub.b], writes=[sub.b])
    pr2 = Tile(nc, "pr2", [128, 2], F32)
    neglam = Tile(nc, "neglam", [128, 1], F32)
    st = mk_ring(nc, "st", 2, [128, 512], F32, psum=True)
    P.op(DVE, lambda e: e.tensor_tensor(pr2[:, 0:1], lamv[:, 0:1], lamv[:, 1:2], op=ALU.mult), reads=[lamv.b], writes=[pr2.b])
    P.op(DVE, lambda e: e.tensor_tensor(pr2[:, 1:2], lamv[:, 2:3], lamv[:, 3:4], op=ALU.mult), reads=[lamv.b, pr2.b], writes=[pr2.b])
    s0 = st.next()
    MM(P, s0[:, 0:2], onesf[:], pr2[:], True, True, [onesf.b, pr2.b], [s0.b])
    P.op(ACT, lambda e: e.activation(pr2[:], s0[:, 0:2], AF.Exp), reads=[s0.b], writes=[pr2.b])
    P.op(DVE, lambda e: e.tensor_tensor(neglam[:], pr2[:, 1:2], pr2[:, 0:1], op=ALU.subtract), reads=[pr2.b], writes=[neglam.b])
    P.op(DVE, lambda e: e.tensor_scalar(neglam[:], neglam[:], -LAMBDA_INIT, None, op0=ALU.add), reads=[neglam.b], writes=[neglam.b])

    kt = Tile(nc, "kt", [128, 2, S], BF16)
    NKC = 4
    kt.bs = [[Buf(f"kt{c}_{i}") for i in range(NKC)] for c in range(2)]
    vt = Tile(nc, "vt", [128, NKB, 256], BF16)
    NVC = 8
    vt.bs = [Buf(f"vt{i}") for i in range(NVC)]
    qr = mk_ring(nc, "q", 2, [128, 2, 512], BF16)
    abr = mk_ring(nc, "ab", 3, [128, 512], F32)
    tr = mk_ring(nc, "t", 3, [128, 512], F32)
    pr = mk_ring(nc, "p", 4, [128, 512], BF16)
    ot = [Tile(nc, f"ot{i}", [128, 512], F32, psum=True) for i in range(4)]
    sm = [Tile(nc, f"sm{i}", [128, 512], F32, psum=True) for i in range(2)]
    rl = [Tile(nc, f"rl{i}", [128, 512], F32) for i in range(2)]
    oc = [Tile(nc, f"oc{i}", [128, 512], F32) for i in range(4)]
    dd = [Tile(nc, f"dd{i}", [128, 512], F32) for i in range(2)]
    sq = [Tile(nc, f"sq{i}", [128, 512], BF16) for i in range(2)]
    rs = Tile(nc, "rs", [128, 512], F32)
    outr = mk_ring(nc, "o", 4, [128, 512], BF16)
    slopes = [2.0 ** (-(h + 1)) for h in range(8)]
    cw = S // NKC
    vb = NKB // NVC
    for h in range(NH):
        qqs = qqr.next()
        rbh = rbr.next()
        P.op(DVE, lambda e, qqs=qqs, h=h: e.tensor_scalar(qqs[:], kq[:, 4, :], slopes[h], None, op0=ALU.mult), reads=[kq.b], writes=[qqs.b])
        P.op(DVE, lambda e, rbh=rbh, h=h: e.tensor_scalar(rbh[:], dt[:, 1, :], -slopes[h], None, op0=ALU.mult), reads=[dt.b], writes=[rbh.b])
        for c in range(2):
            for i in range(NKC):
                P.dma(SP, lambda e, i=i, c=c, h=h: e.dma_start(out=kt[:, c, i * cw:(i + 1) * cw], in_=KT[2 * h + c, :, i * cw:(i + 1) * cw]),
                      kt.bs[c][i], writes=[kt.bs[c][i]])
        for i in range(NVC):
            P.dma(SP, lambda e, i=i, h=h: e.dma_start(
                out=vt[:, i * vb:(i + 1) * vb, :],
                in_=V[i * vb * 128:(i + 1) * vb * 128, h * 256:(h + 1) * 256].rearrange("(kb p) d -> p kb d", p=128)),
                vt.bs[i], writes=[vt.bs[i]])
        for j in range(nj):
            t0 = j * 512
            q = qr.next()
            P.dma(SP, lambda e, q=q, t0=t0, h=h: e.dma_start(out=q[:], in_=QT[2 * h:2 * h + 2, :, t0:t0 + 512].rearrange("c p t -> p c t")),
                  q.b, writes=[q.b])

            def qk(kb, c, ab):
                s = st.next()
                MM(P, s[:], kt[:, c, kb * 128:(kb + 1) * 128], q[:, c, :], True, True, [kt.bs[c][kb * 128 // cw], q.b], [s.b])
                t = tr.next()
                p = pr.next()
                o = (kb - 4 * j) % NKB
                idx = j * NKB + kb
                if o < 4:
                    P.op(DVE, lambda e, t=t, s=s, o=o, h=h: e.scalar_tensor_tensor(out=t[:], in0=kq[:, o, :], scalar=-slopes[h], in1=s[:],
                                                                                op0=ALU.mult, op1=ALU.add),
                         reads=[kq.b, s.b], writes=[t.b])
                    P.op(ACT, lambda e, p=p, t=t: e.activation(p[:], t[:], AF.Exp), reads=[t.b], writes=[p.b])
                else:
                    P.op(DVE, lambda e, t=t, s=s, idx=idx, qqs=qqs: e.scalar_tensor_tensor(out=t[:], in0=qqs[:], scalar=dt[:, 0, idx:idx + 1],
                                                                                        in1=s[:], op0=ALU.mult, op1=ALU.add),
                         reads=[qqs.b, dt.b, s.b], writes=[t.b])
                    P.op(ACT, lambda e, p=p, t=t, idx=idx, rbh=rbh: e.activation(p[:], t[:], AF.Exp, bias=rbh[:, idx:idx + 1]),
                         reads=[t.b, rbh.b], writes=[p.b])
                return p

            def mkab(kb):
                return None

            def pv(kb, c, p, first, last):
                MM(P, ot[2 * c][:], vt[:, kb, 0:128], p[:], first, last, [vt.bs[kb // vb], p.b], [ot[2 * c].b])
                MM(P, ot[2 * c + 1][:], vt[:, kb, 128:256], p[:], first, last, [vt.bs[kb // vb], p.b], [ot[2 * c + 1].b])
                MM(P, sm[c][:], ones[:], p[:], first, last, [ones.b, p.b], [sm[c].b])

            if win is None or 2 * win[h] + 4 >= NKB:
                slots = list(range(NKB))
            else:
                slots = [(4 * j + o) % NKB for o in range(-win[h], 4 + win[h])]
            ns = len(slots)
            ab = mkab(slots[0])
            pend = [qk(slots[0], 0, ab), qk(slots[0], 1, ab)]
            for si in range(ns):
                kb = slots[si]
                nxt = [None, None]
                for c in range(2):
                    if si + 1 < ns:
                        nxt[c] = qk(slots[si + 1], c, None)
                    pv(kb, c, pend[c], si == 0, si == ns - 1)
                pend = nxt
            for c in range(2):
                P.op(DVE, lambda e, c=c: e.reciprocal(rl[c][:], sm[c][:]), reads=[sm[c].b], writes=[rl[c].b])
                for hf in range(2):
                    i = 2 * c + hf
                    P.op(DVE, lambda e, i=i, c=c: e.tensor_tensor(oc[i][:], ot[i][:], rl[c][:], op=ALU.mult),
                         reads=[ot[i].b, rl[c].b], writes=[oc[i].b])
            ssb = st.next()
            for hf in range(2):
                P.op(DVE, lambda e, hf=hf: e.scalar_tensor_tensor(out=dd[hf][:], in0=oc[2 + hf][:], scalar=neglam[:, 0:1], in1=oc[hf][:],
                                                                 op0=ALU.mult, op1=ALU.add),
                     reads=[oc[2 + hf].b, oc[hf].b, neglam.b], writes=[dd[hf].b])
                P.op(ACT, lambda e, hf=hf: e.activation(sq[hf][:], dd[hf][:], AF.Square), reads=[dd[hf].b], writes=[sq[hf].b])
                MM(P, ssb[:], ones[:], sq[hf][:], hf == 0, hf == 1, [ones.b, sq[hf].b], [ssb.b])
            P.op(ACT, lambda e, ssb=ssb: e.activation(rs[:], ssb[:], AF.Sqrt, bias=epsb[:, 0:1], scale=1.0 / 256),
                 reads=[ssb.b, epsb.b], writes=[rs.b])
            P.op(DVE, lambda e: e.reciprocal(rs[:], rs[:]), reads=[rs.b], writes=[rs.b])
            for hf in range(2):
                o = outr.next()
                P.op(DVE, lambda e, o=o, hf=hf: e.scalar_tensor_tensor(out=o[:], in0=dd[hf][:], scalar=sub[:, hf:hf + 1], in1=rs[:],
                                                                      op0=ALU.mult, op1=ALU.mult),
                     reads=[dd[hf].b, sub.b, rs.b], writes=[o.b])
                P.dma(SP, lambda e, o=o, t0=t0, hf=hf, h=h: e.dma_start(out=OT[2 * h + hf, :, t0:t0 + 512], in_=o[:]), o.b, reads=[o.b])
    P.emit()
    return nc


def k3_program(T, NF, mode, TT=512):
    nc = bass.Bass("TRN2", target_bir_lowering=False)
    if mode != "ffn":
        X = nc.dram_tensor("x", [T, 2048], F32, kind="ExternalInput").ap()
        OTd = nc.dram_tensor("oT", [16, 128, T], BF16, kind="ExternalInput").ap()
        WO = nc.dram_tensor("wo", [2048, 2048], F32, kind="ExternalInput").ap()
        G = nc.dram_tensor("g", [2048], F32, kind="ExternalInput").ap()
    if mode != "router":
        WG = nc.dram_tensor("wg", [1, 2048, NF], F32, kind="ExternalInput").ap()
        WU = nc.dram_tensor("wu", [1, 2048, NF], F32, kind="ExternalInput").ap()
        WD = nc.dram_tensor("wd", [1, NF, 2048], F32, kind="ExternalInput").ap()
    if mode == "router":
        WR = nc.dram_tensor("wr", [2048, 8], F32, kind="ExternalInput").ap()
        HBo = nc.dram_tensor("hb", [T, 2048], BF16, kind="ExternalOutput").ap()
        GTo = nc.dram_tensor("gate", [T, 8], F32, kind="ExternalOutput").ap()
    if mode == "ffn":
        HBi = nc.dram_tensor("hb", [T, 2048], BF16, kind="ExternalInput").ap()
    Y = nc.dram_tensor("y", [T, 2048], F32, kind="ExternalOutput").ap()
    P = Prog(nc)
    identf, ident, ones = build_consts(nc, P)
    C = {"ident": ident, "junk": Tile(nc, "junk", [128, 2048], BF16), "ss": Tile(nc, "ss", [128, 4], F32),
         "rstd": Tile(nc, "rstd", [128, 4], F32), "epsb": Tile(nc, "epsb", [128, 1], F32)}
    P.op(DVE, lambda e: e.memset(C["epsb"][:], EPS), writes=[C["epsb"].b])
    nsub = TT // 128
    NFC = NF // 128
    nb = NFC // 4
    hb = Tile(nc, "hb", [128, nsub, 2048], BF16)
    hT = Tile(nc, "hT", [128, 16, TT], BF16)
    hT.bs = [Buf(f"hT{c}") for c in range(16)]
    wr_ = mk_ring(nc, "wblk", 3, [128, 16, 512], BF16)
    ptr = mk_ring(nc, "pt", 2, [128, 512], BF16, psum=True)
    psr = mk_ring(nc, "ps", 4, [128, 512], F32, psum=True)
    xt = Tile(nc, "xt", [128, nsub, 2048], F32)
    if mode != "ffn":
        gB = Tile(nc, "gB", [128, 2048], F32)
        P.dma(SP, lambda e: e.dma_start(out=gB[:], in_=G.partition_broadcast(128)), gB.b, writes=[gB.b])
    if mode != "router":
        A = Tile(nc, "A", [128, NFC, TT], BF16)
        A.bs = [Buf(f"A{c}") for c in range(NFC)]
        sgr = mk_ring(nc, "sg", 3, [128, TT], F32)
    if mode == "router":
        wrt = Tile(nc, "wrt", [128, 16, 8], F32)
        P.dma(SP, lambda e: e.dma_start(out=wrt[:], in_=WR.rearrange("(c p) n -> p c n", p=128)), wrt.b, writes=[wrt.b])
        hf = Tile(nc, "hf", [128, 2048], F32)
        hTf = Tile(nc, "hTf", [128, 16, 128], F32)
        pf = Tile(nc, "pf", [128, 512], F32, psum=True)
        pl = Tile(nc, "pl", [128, 512], F32, psum=True)
        lg = Tile(nc, "lg", [128, 8], F32)
        m8 = Tile(nc, "m8", [128, 8], F32)
        gs = Tile(nc, "gs", [128, 4], F32)
        eq = Tile(nc, "eq", [128, 16], F32)
        gate = Tile(nc, "gate", [128, nsub, 8], F32)

    def wload(src_ap, nchunk):
        wb = wr_.next()
        P.dma(POOL, lambda e, wb=wb: e.dma_start(out=wb[:, 0:nchunk, :], in_=src_ap), wb.b, writes=[wb.b])
        return wb

    for tt in range(T // TT):
        t0 = tt * TT
        if mode == "ffn":
            P.dma(SP, lambda e, t0=t0: e.dma_start(out=hb[:], in_=HBi[t0:t0 + TT, :].rearrange("(s p) d -> p s d", p=128)),
                  hb.b, writes=[hb.b])
            transpose_to_fm(P, C, hb, hT, ptr, nsub)
        else:
            P.dma(SP, lambda e, t0=t0: e.dma_start(out=xt[:], in_=X[t0:t0 + TT, :].rearrange("(s p) d -> p s d", p=128)),
                  xt.b, writes=[xt.b])
            P.dma(SP, lambda e, t0=t0: e.dma_start(out=hT[:], in_=OTd[:, :, t0:t0 + TT].rearrange("c p t -> p c t")),
                  hT.bs[0], writes=list(hT.bs))
            for cg in range(4):
                wb = wload(WO[:, cg * 512:(cg + 1) * 512].rearrange("(c p) n -> p c n", p=128), 16)
                for s in range(nsub):
                    ps = psr.next()
                    for c in range(16):
                        MM(P, ps[:], hT[:, c, s * 128:(s + 1) * 128], wb[:, c, :], c == 0, c == 15, [wb.b, hT.bs[c]], [ps.b])
                    P.op(DVE, lambda e, ps=ps, s=s, cg=cg: e.tensor_tensor(xt[:, s, cg * 512:(cg + 1) * 512], xt[:, s, cg * 512:(cg + 1) * 512],
                                                                         ps[:], op=ALU.add), reads=[ps.b, xt.b], writes=[xt.b])
            norm_transpose(P, nc, C, xt, gB, hb, hT, ptr, nsub)
        if mode == "router":
            for s in range(nsub):
                P.op(DVE, lambda e, s=s: e.scalar_tensor_tensor(out=hf[:], in0=xt[:, s, :], scalar=C["rstd"][:, s:s + 1], in1=gB[:],
                                                             op0=ALU.mult, op1=ALU.mult), reads=[xt.b, C["rstd"].b, gB.b], writes=[hf.b])
                for g4 in range(4):
                    for i in range(4):
                        c = g4 * 4 + i
                        P.op(PE, lambda e, c=c, i=i: e.transpose(pf[:, i * 128:(i + 1) * 128], hf[:, c * 128:(c + 1) * 128], identf[:]),
                             reads=[hf.b, identf.b], writes=[pf.b])
                    P.op(ACT, lambda e, g4=g4: e.copy(hTf[:, g4 * 4:(g4 + 1) * 4, :], pf[:].rearrange("p (a b) -> p a b", a=4)),
                         reads=[pf.b], writes=[hTf.b])
                for c in range(16):
                    MM(P, pl[:, 0:8], hTf[:, c, :], wrt[:, c, :], c == 0, c == 15, [hTf.b, wrt.b], [pl.b])
                P.op(DVE, lambda e: e.tensor_copy(lg[:], pl[:, 0:8]), reads=[pl.b], writes=[lg.b])
                P.op(DVE, lambda e: e.max(m8[:], lg[:]), reads=[lg.b], writes=[m8.b])
                P.op(DVE, lambda e: e.tensor_tensor(gs[:, 0:1], m8[:, 1:2], m8[:, 0:1], op=ALU.subtract), reads=[m8.b], writes=[gs.b])
                P.op(ACT, lambda e: e.activation(gs[:, 0:1], gs[:, 0:1], AF.Exp), reads=[gs.b], writes=[gs.b])
                P.op(DVE, lambda e: e.tensor_scalar(gs[:, 1:2], gs[:, 0:1], 1.0, None, op0=ALU.add), reads=[gs.b], writes=[gs.b])
                P.op(DVE, lambda e: e.reciprocal(gs[:, 1:2], gs[:, 1:2]), reads=[gs.b], writes=[gs.b])
                P.op(DVE, lambda e: e.tensor_tensor(gs[:, 2:3], gs[:, 0:1], gs[:, 1:2], op=ALU.mult), reads=[gs.b], writes=[gs.b])
                P.op(DVE, lambda e: e.tensor_scalar(eq[:, 0:8], lg[:], m8[:, 0:1], gs[:, 1:2], op0=ALU.is_equal, op1=ALU.mult),
                     reads=[lg.b, m8.b, gs.b], writes=[eq.b])
                P.op(DVE, lambda e: e.tensor_scalar(eq[:, 8:16], lg[:], m8[:, 1:2], gs[:, 2:3], op0=ALU.is_equal, op1=ALU.mult),
                     reads=[lg.b, m8.b, gs.b, eq.b], writes=[eq.b])
                P.op(DVE, lambda e, s=s: e.tensor_tensor(gate[:, s, :], eq[:, 0:8], eq[:, 8:16], op=ALU.add), reads=[eq.b], writes=[gate.b])
            P.dma(SP, lambda e, t0=t0: e.dma_start(out=HBo[t0:t0 + TT, :].rearrange("(s p) d -> p s d", p=128), in_=hb[:]),
                  hb.b, reads=[hb.b])
            P.dma(SP, lambda e, t0=t0: e.dma_start(out=GTo[t0:t0 + TT, :].rearrange("(s p) d -> p s d", p=128), in_=gate[:]),
                  gate.b, reads=[gate.b])
        else:
            for fb in range(NF // 512):
                wgb = wload(WG[0, :, fb * 512:(fb + 1) * 512].rearrange("(c p) n -> p c n", p=128), 16)
                wub = wload(WU[0, :, fb * 512:(fb + 1) * 512].rearrange("(c p) n -> p c n", p=128), 16)
                for fc in range(4):
                    pg = psr.next()
                    for c in range(16):
                        MM(P, pg[:, 0:TT], wgb[:, c, fc * 128:(fc + 1) * 128], hT[:, c, :], c == 0, c == 15, [wgb.b, hT.bs[c]], [pg.b])
                    pu = psr.next()
                    for c in range(16):
                        MM(P, pu[:, 0:TT], wub[:, c, fc * 128:(fc + 1) * 128], hT[:, c, :], c == 0, c == 15, [wub.b, hT.bs[c]], [pu.b])
                    sg = sgr.next()
                    P.op(ACT, lambda e, sg=sg, pg=pg: e.activation(sg[:], pg[:, 0:TT], AF.Silu), reads=[pg.b], writes=[sg.b])
                    ac = fb * 4 + fc
                    P.op(DVE, lambda e, sg=sg, pu=pu, ac=ac: e.tensor_tensor(A[:, ac, :], sg[:], pu[:, 0:TT], op=ALU.mult),
                         reads=[sg.b, pu.b], writes=[A.bs[ac]])
            for cg in range(4):
                accs = [psr.next() for _ in range(nsub)]
                for rb in range(4):
                    wdb = wload(WD[0, rb * nb * 128:(rb + 1) * nb * 128, cg * 512:(cg + 1) * 512].rearrange("(c p) n -> p c n", p=128), nb)
                    for s in range(nsub):
                        for c in range(nb):
                            ac = rb * nb + c
                            MM(P, accs[s][:], A[:, ac, s * 128:(s + 1) * 128], wdb[:, c, :], rb == 0 and c == 0, rb == 3 and c == nb - 1,
                               [wdb.b, A.bs[ac]], [accs[s].b])
                for s in range(nsub):
                    acc = accs[s]
                    if mode == "ffn":
                        if s % 2 == 0:
                            P.op(ACT, lambda e, acc=acc, s=s, cg=cg: e.copy(xt[:, s, cg * 512:(cg + 1) * 512], acc[:]),
                                 reads=[acc.b, xt.b], writes=[xt.b])
                        else:
                            P.op(DVE, lambda e, acc=acc, s=s, cg=cg: e.tensor_copy(xt[:, s, cg * 512:(cg + 1) * 512], acc[:]),
                                 reads=[acc.b, xt.b], writes=[xt.b])
                    else:
                        P.op(DVE, lambda e, acc=acc, s=s, cg=cg: e.tensor_tensor(
                            xt[:, s, cg * 512:(cg + 1) * 512], xt[:, s, cg * 512:(cg + 1) * 512], acc[:], op=ALU.add),
                            reads=[acc.b, xt.b], writes=[xt.b])
        P.dma(SP, lambda e, t0=t0: e.dma_start(out=Y[t0:t0 + TT, :].rearrange("(s p) d -> p s d", p=128), in_=xt[:]),
              xt.b, reads=[xt.b])
    P.emit()
    return nc


def combine_program(T, TT=512):
    nc = bass.Bass("TRN2", target_bir_lowering=False)
    X = nc.dram_tensor("x", [T, 2048], F32, kind="ExternalInput").ap()
    Y1 = nc.dram_tensor("y1", [T, 2048], F32, kind="ExternalInput").ap()
    Y2 = nc.dram_tensor("y2", [T, 2048], F32, kind="ExternalInput").ap()
    GG = nc.dram_tensor("g2", [T, 2], F32, kind="ExternalInput").ap()
    Y = nc.dram_tensor("y", [T, 2048], F32, kind="ExternalOutput").ap()
    P = Prog(nc)
    nsub = TT // 128
    xr = mk_ring(nc, "x", 2, [128, nsub, 2048], F32)
    ar = mk_ring(nc, "a", 2, [128, nsub, 2048], F32)
    gr = mk_ring(nc, "g", 2, [128, nsub, 2], F32)
    for tt in range(T // TT):
        t0 = tt * TT
        xt, gt = xr.next(), gr.next()
        P.dma(SP, lambda e, t0=t0, xt=xt: e.dma_start(out=xt[:], in_=X[t0:t0 + TT, :].rearrange("(s p) d -> p s d", p=128)), xt.b, writes=[xt.b])
        P.dma(SP, lambda e, t0=t0, gt=gt: e.dma_start(out=gt[:], in_=GG[t0:t0 + TT, :].rearrange("(s p) d -> p s d", p=128)), gt.b, writes=[gt.b])
        for k, src in enumerate((Y1, Y2)):
            at = ar.next()
            P.dma(SP, lambda e, t0=t0, at=at, src=src: e.dma_start(out=at[:], in_=src[t0:t0 + TT, :].rearrange("(s p) d -> p s d", p=128)),
                  at.b, writes=[at.b])
            for s in range(nsub):
                P.op(DVE, lambda e, at=at, xt=xt, gt=gt, s=s, k=k: e.scalar_tensor_tensor(
                    out=xt[:, s, :], in0=at[:, s, :], scalar=gt[:, s, k:k + 1], in1=xt[:, s, :], op0=ALU.mult, op1=ALU.add),
                    reads=[at.b, gt.b, xt.b], writes=[xt.b])
        P.dma(SP, lambda e, t0=t0, xt=xt: e.dma_start(out=Y[t0:t0 + TT, :].rearrange("(s p) d -> p s d", p=128), in_=xt[:]), xt.b, reads=[xt.b])
    P.emit()
    return nc


import ml_dtypes

NCORES = 8
TPC = 4096
SEQ = 16384
L1_WIN = [min(128, int(np.ceil((155.0 / 2.0 ** (-(h + 1))) / 128.0)) + 1) for h in range(8)]


def _run(nc, in_maps):
    res = run_bass_kernel_spmd(nc, in_maps, core_ids=list(range(NCORES)))
    return res.results


def _gather_batch(rs, key, axis):
    out = []
    for b in range(2):
        out.append(np.concatenate([rs[4 * b + q][key] for q in range(4)], axis=axis))
    return out


def kernel(**inp):
    f32 = np.float32
    x = np.ascontiguousarray(inp["x"], dtype=f32).reshape(NCORES * TPC, 2048)
    xs = [x[c * TPC:(c + 1) * TPC] for c in range(NCORES)]
    tabs = [rope_tables((c % 4) * TPC + np.arange(TPC)) for c in range(4)]
    nc = k1_program(TPC, **l0_spec())
    r1 = _run(nc, [dict(x=xs[c], g=inp["l0_norm_mix"], w=inp["l0_w_in"], gq=inp["l0_qnorm_a"], gk=inp["l0_knorm_a"],
                        gq2=inp["l0_qnorm_b"], gk2=inp["l0_knorm_b"], cosT=tabs[c % 4][0], sinT=tabs[c % 4][1], rt=tabs[c % 4][2])
                   for c in range(NCORES)])
    kT_b = _gather_batch(r1, "kT", 2)
    v_b = _gather_batch(r1, "v", 0)
    biasB = dil_bias_tables()
    SL = TPC + 2048
    maps = []
    for c in range(NCORES):
        b, q = c // 4, c % 4
        lo = q * TPC - 1024
        kTl = np.zeros((8, 128, SL), ml_dtypes.bfloat16)
        vl = np.zeros((SL, 1024), ml_dtypes.bfloat16)
        kmask = np.full((128, SL // 128), -30000.0, f32)
        a0, a1 = max(lo, 0), min(lo + SL, SEQ)
        kTl[:, :, a0 - lo:a1 - lo] = kT_b[b][2:10, :, a0:a1]
        vl[a0 - lo:a1 - lo] = v_b[b][a0:a1, 256:1280]
        kmask[:, (a0 - lo) // 128:(a1 - lo) // 128] = 0.0
        maps.append(dict(qT=r1[c]["qT"], kT=np.ascontiguousarray(kT_b[b][0:2]), v=np.ascontiguousarray(v_b[b][:, 0:256]),
                         kTl=kTl, vl=vl, kmask=kmask, biasB=biasB))
    nc = k2_l0_program(TPC, SEQ, SL)
    r2 = _run(nc, maps)
    nc = k3_program(TPC, 5632, "dense")
    r3 = _run(nc, [dict(x=xs[c], oT=r2[c]["oT"], wo=inp["l0_w_out"], g=inp["l0_norm_ffn"], wg=inp["l0_w_gate"][None],
                        wu=inp["l0_w_up"][None], wd=inp["l0_w_down"][None]) for c in range(NCORES)])
    x1 = [r3[c]["y"] for c in range(NCORES)]
    nc = k1_program(TPC, **l1_spec())
    r4 = _run(nc, [dict(x=x1[c], g=inp["l1_norm_mix"], w=inp["l1_w_in"], gq=inp["l1_qnorm_c"], gk=inp["l1_knorm_c"])
                   for c in range(NCORES)])
    kT_b = _gather_batch(r4, "kT", 2)
    v_b = _gather_batch(r4, "v", 0)
    KQ = np.zeros((128, 5, 512), f32)
    for o in range(4):
        KQ[:, o, :] = np.abs(128 * o + np.arange(128)[:, None] - np.arange(512)[None, :])
    KQ[:, 4, :] = np.arange(512)[None, :]
    lamv = np.stack([inp["l1_lambda_q1"], inp["l1_lambda_k1"], inp["l1_lambda_q2"], inp["l1_lambda_k2"]], axis=1).astype(f32)
    maps = []
    for c in range(NCORES):
        b, q = c // 4, c % 4
        DT = np.zeros((128, 2, 8 * 128), f32)
        kbs = (np.arange(128) + 32 * q) % 128
        for j in range(8):
            delta = kbs * 128 - (q * TPC + j * 512)
            DT[:, 0, j * 128:(j + 1) * 128] = np.where(delta >= 512, 1.0, -1.0)[None, :]
            DT[:, 1, j * 128:(j + 1) * 128] = np.abs(delta[None, :] + np.arange(128)[:, None])
        maps.append(dict(qT=r4[c]["qT"], kT=np.roll(kT_b[b], -q * TPC, axis=2), v=np.roll(v_b[b], -q * TPC, axis=0),
                         DT=DT, KQ=KQ, lamv=np.ascontiguousarray(lamv), subln=inp["l1_subln"]))
    nc = k2_l1_program(TPC, SEQ, win=L1_WIN)
    r5 = _run(nc, maps)
    nc = k3_program(TPC, 7168, "router")
    r6 = _run(nc, [dict(x=x1[c], oT=r5[c]["oT"], wo=inp["l1_w_out"], g=inp["l1_norm_ffn"], wr=inp["l1_w_router"])
                   for c in range(NCORES)])
    H = np.concatenate([r6[c]["hb"] for c in range(NCORES)], axis=0)
    GT = np.concatenate([r6[c]["gate"] for c in range(NCORES)], axis=0)
    sel = GT != 0
    toks = [np.nonzero(sel[:, e])[0] for e in range(8)]
    cap = max(512, int(-(-max(len(t) for t in toks) // 512) * 512))
    maps = []
    for e in range(8):
        he = np.zeros((cap, 2048), ml_dtypes.bfloat16)
        he[:len(toks[e])] = H[toks[e]]
        maps.append(dict(hb=he, wg=inp["l1_e_gate"][e:e + 1], wu=inp["l1_e_up"][e:e + 1], wd=inp["l1_e_down"][e:e + 1]))
    nc = k3_program(cap, 7168, "ffn")
    r7 = _run(nc, maps)
    NT = NCORES * TPC
    slot = np.zeros((NT, 8), np.int64)
    for e in range(8):
        slot[toks[e], e] = np.arange(len(toks[e]))
    order = np.argsort(~sel, axis=1, kind="stable")[:, :2]
    ys, g2 = [], np.take_along_axis(GT, order, axis=1).astype(f32)
    for k in range(2):
        yk = np.zeros((NT, 2048), f32)
        for e in range(8):
            m = order[:, k] == e
            yk[m] = r7[e]["y"][slot[m, e]]
        ys.append(yk)
    nc = combine_program(TPC)
    r6 = _run(nc, [dict(x=r6[c]["y"], y1=ys[0][c * TPC:(c + 1) * TPC], y2=ys[1][c * TPC:(c + 1) * TPC],
                        g2=np.ascontiguousarray(g2[c * TPC:(c + 1) * TPC])) for c in range(NCORES)])
    out = np.concatenate([r6[c]["y"] for c in range(NCORES)], axis=0).reshape(2, SEQ, 2048).astype(f32)
    return out
```
